# Optimizing a Trainium2 kernel written in Bass

```python
import math
import jax, jax.numpy as jnp
from jax import lax
import numpy as np

D_MODEL = 1024
BATCH = 2
SEQ = 8192
DEPTH = 4

CTX_LEN = 256
GRID_W = 64
GDN_H = 4
GDN_DK = 128
GDN_DV = 128
GDN_CHUNK = 64
CONV_K = 3
DIFF_H = 4
DIFF_D = 64
Q_BLOCK = 128
ROPE_BASE = 10000.0
NA_H = 8
NA_D = 64
WIN_H = 8
WIN_W = 16
NA_QB_W = 16
NA_KB_W = 32
N_BRANCH = 3
BRANCH_W = 512
GDN_QK = GDN_H * GDN_DK
GDN_V = GDN_H * GDN_DV
DIFF_W = 2 * DIFF_H * DIFF_D
NA_W = NA_H * NA_D
IN_SPLITS = (GDN_QK, GDN_QK, GDN_V, GDN_V, GDN_H, GDN_H, GDN_H, GDN_H,
             DIFF_W, DIFF_W, DIFF_W, NA_W, NA_W, NA_W, N_BRANCH * D_MODEL)
D_IN = sum(IN_SPLITS)
N_EXPERTS = 32
TOP_K = 4
D_EXPERT = 1024
SWIGLU_LIMIT = 7.0
SWIGLU_ALPHA = 1.702
MOE_BLOCK = 128
DN_ALPHA = (2 * DEPTH) ** 0.25
DN_BETA = (8 * DEPTH) ** -0.25
LN_EPS = 1e-5
RMS_EPS = 1e-6
NEG_INF = -1e30

kernel_name = 'hybrid_gdn_diffattn_natten_moe_dit'

F32 = jnp.float32


def _layernorm(x):
    xf = x.astype(F32)
    mu = jnp.mean(xf, -1, keepdims=True)
    var = jnp.mean(jnp.square(xf - mu), -1, keepdims=True)
    return ((xf - mu) * lax.rsqrt(var + LN_EPS)).astype(x.dtype)


def _rmsnorm(x, w):
    xf = x.astype(F32)
    return xf * lax.rsqrt(jnp.mean(jnp.square(xf), -1, keepdims=True) + RMS_EPS) * w.astype(F32)


def _l2norm(x):
    return x * lax.rsqrt(jnp.sum(jnp.square(x), -1, keepdims=True) + RMS_EPS)


def _modulate(x, shift, scale):
    return _layernorm(x) * (1 + scale) + shift


def _post_norm(x, y, g, b):
    return _layernorm(DN_ALPHA * x + y) * g + b


def _split_cols(p):
    return jnp.split(p, np.cumsum(IN_SPLITS)[:-1].tolist(), axis=-1)


def _axial_rope(n_tok, dtype):
    t = jnp.arange(n_tok)
    row = (t // GRID_W).astype(F32)
    col = (t % GRID_W).astype(F32)
    n_freq = DIFF_D // 4
    inv = ROPE_BASE ** (-jnp.arange(n_freq, dtype=F32) / n_freq)
    ang = jnp.concatenate([row[:, None] * inv, col[:, None] * inv], -1)
    ang = jnp.concatenate([ang, ang], -1)
    return jnp.cos(ang).astype(dtype), jnp.sin(ang).astype(dtype)


def _apply_rope(x, cos, sin):
    x1, x2 = jnp.split(x, 2, axis=-1)
    return x * cos + jnp.concatenate([-x2, x1], -1) * sin


def _short_conv(x, w):
    return lax.conv_general_dilated(x, w[:, None, :], window_strides=(1,),
                                    padding=[(CONV_K // 2, CONV_K // 2)],
                                    dimension_numbers=('NWC', 'WIO', 'NWC'),
                                    feature_group_count=x.shape[-1])


def _gdn_qkv(q, k, v, conv_w):
    b, L, _ = q.shape
    qkv = jax.nn.silu(_short_conv(jnp.concatenate([q, k, v], -1), conv_w)).astype(F32)
    q, k, v = jnp.split(qkv, [GDN_QK, 2 * GDN_QK], axis=-1)
    q = _l2norm(q.reshape(b, L, GDN_H, GDN_DK)) * GDN_DK ** -0.5
    k = _l2norm(k.reshape(b, L, GDN_H, GDN_DK))
    return q, k, v.reshape(b, L, GDN_H, GDN_DV)


def _gdn_decay(a, bb, a_log, dt_bias):
    g = -jnp.exp(a_log.astype(F32)) * jax.nn.softplus(a.astype(F32) + dt_bias.astype(F32))
    return g, jax.nn.sigmoid(bb.astype(F32))


def _gdn_chunk_scan(q, k, v, g, beta, s0, with_out):
    b, L, h, _ = q.shape
    dv = v.shape[-1]
    cs = GDN_CHUNK
    n = L // cs

    def blk(t):
        return t.reshape(b, n, cs, h, -1).transpose(0, 3, 1, 2, 4)

    q, k, v = blk(q), blk(k), blk(v)
    g = g.reshape(b, n, cs, h).transpose(0, 3, 1, 2)
    beta = beta.reshape(b, n, cs, h).transpose(0, 3, 1, 2)
    gc = jnp.cumsum(g, axis=-1)
    i = jnp.arange(cs)
    incl = i[:, None] >= i[None, :]
    strict = i[:, None] > i[None, :]
    decay = jnp.where(incl, jnp.exp(jnp.where(incl, gc[..., :, None] - gc[..., None, :], 0.0)), 0.0)
    kb = k * beta[..., None]
    low = jnp.where(strict, jnp.einsum('bhnid,bhnjd->bhnij', kb, k) * decay, 0.0)
    rhs = jnp.concatenate([v * beta[..., None], kb * jnp.exp(gc)[..., None]], axis=-1)
    sol = lax.linalg.triangular_solve(low + jnp.eye(cs, dtype=F32), rhs, left_side=True, lower=True)
    u, w = sol[..., :dv], sol[..., dv:]
    g_last = gc[..., -1]
    k_tail = k * jnp.exp(g_last[..., None] - gc)[..., None]

    def seq(t):
        return jnp.moveaxis(t, 2, 0)

    def state_update(S, u_i, w_i, kt_i, gl_i):
        v_new = u_i - jnp.einsum('bhck,bhkv->bhcv', w_i, S)
        S_new = S * jnp.exp(gl_i)[..., None, None] + jnp.einsum('bhck,bhcv->bhkv', kt_i, v_new)
        return S_new, v_new

    if not with_out:
        def step(S, xs):
            S_new, _ = state_update(S, *xs)
            return S_new, None
        s_fin, _ = lax.scan(step, s0, (seq(u), seq(w), seq(k_tail), seq(g_last)))
        return s_fin, None

    qk = jnp.where(incl, jnp.einsum('bhnid,bhnjd->bhnij', q, k) * decay, 0.0)
    qg = q * jnp.exp(gc)[..., None]

    def step_out(S, xs):
        u_i, w_i, kt_i, gl_i, qk_i, qg_i = xs
        S_new, v_new = state_update(S, u_i, w_i, kt_i, gl_i)
        o_i = jnp.einsum('bhck,bhkv->bhcv', qg_i, S) + jnp.einsum('bhcj,bhjv->bhcv', qk_i, v_new)
        return S_new, o_i

    s_fin, o = lax.scan(step_out, s0, tuple(seq(t) for t in (u, w, k_tail, g_last, qk, qg)))
    o = jnp.moveaxis(o, 0, 2).transpose(0, 2, 3, 1, 4).reshape(b, L, h, dv)
    return s_fin, o


def _gated_rms(o, z, w):
    b, L, h, dv = o.shape
    zf = z.reshape(b, L, h, dv).astype(F32)
    return (_rmsnorm(o, w) * jax.nn.silu(zf)).reshape(b, L, h * dv).astype(z.dtype)


def _gdn_branch(lat, ctx, conv_w, a_log, dt_bias, norm_w, with_ctx_out):
    qx, kx, vx = _gdn_qkv(lat[0], lat[1], lat[2], conv_w)
    qc, kc, vc = _gdn_qkv(ctx[0], ctx[1], ctx[2], conv_w)
    s0 = jnp.zeros((qx.shape[0], GDN_H, GDN_DK, GDN_DV), F32)
    outs_x, outs_c = [], []
    for d in range(2):
        fl = (lambda t: t[:, ::-1]) if d else (lambda t: t)
        gx, bx = _gdn_decay(lat[4 + 2 * d], lat[5 + 2 * d], a_log[d], dt_bias[d])
        gcc, bcc = _gdn_decay(ctx[4 + 2 * d], ctx[5 + 2 * d], a_log[d], dt_bias[d])
        s_ctx, oc = _gdn_chunk_scan(fl(qc), fl(kc), fl(vc), fl(gcc), fl(bcc), s0, with_ctx_out)
        _, ox = _gdn_chunk_scan(fl(qx), fl(kx), fl(vx), fl(gx), fl(bx), s_ctx, True)
        outs_x.append(fl(ox))
        if with_ctx_out:
            outs_c.append(fl(oc))
    yx = _gated_rms(outs_x[0] + outs_x[1], lat[3], norm_w)
    yc = _gated_rms(outs_c[0] + outs_c[1], ctx[3], norm_w) if with_ctx_out else None
    return yx, yc


def _diff_attend(q, k, v, lam):
    s = jnp.einsum('bhmqd,bhmkd->bhmqk', q, k).astype(F32) * DIFF_D ** -0.5
    p = jax.nn.softmax(s, axis=-1)
    a = p[:, :, 0] - lam * p[:, :, 1]
    return jnp.einsum('bhqk,bhke->bhqe', a.astype(v.dtype), v)


def _diff_out(o, norm_w, lam_init, dtype):
    b, h, L, e = o.shape
    y = _rmsnorm(o, norm_w) * (1.0 - lam_init)
    return y.transpose(0, 2, 1, 3).reshape(b, L, h * e).astype(dtype)


def _diff_branch(lat, ctx, lam_vec, lam_init, norm_w, cos, sin, with_ctx_out):
    qx, kx, vx = lat
    qc, kc, vc = ctx
    b, S, _ = qx.shape

    def qk_heads(t):
        return t.reshape(b, t.shape[1], DIFF_H, 2, DIFF_D).transpose(0, 2, 3, 1, 4)

    def v_heads(t):
        return t.reshape(b, t.shape[1], DIFF_H, 2 * DIFF_D).transpose(0, 2, 1, 3)

    lv = lam_vec.astype(F32)
    lam = jnp.exp(jnp.sum(lv[0] * lv[1])) - jnp.exp(jnp.sum(lv[2] * lv[3])) + lam_init
    qx_h = _apply_rope(qk_heads(qx), cos, sin)
    kx_h = _apply_rope(qk_heads(kx), cos, sin)
    kc_h, vc_h = qk_heads(kc), v_heads(vc)
    k_all = jnp.concatenate([kx_h, kc_h], axis=3)
    v_all = jnp.concatenate([v_heads(vx), vc_h], axis=2)
    nb = S // Q_BLOCK
    q_blocks = jnp.moveaxis(qx_h.reshape(b, DIFF_H, 2, nb, Q_BLOCK, DIFF_D), 3, 0)
    o = lax.map(lambda qb: _diff_attend(qb, k_all, v_all, lam), q_blocks)
    o = jnp.moveaxis(o, 0, 2).reshape(b, DIFF_H, S, 2 * DIFF_D)
    yx = _diff_out(o, norm_w, lam_init, qx.dtype)
    yc = _diff_out(_diff_attend(qk_heads(qc), kc_h, vc_h, lam), norm_w, lam_init, qc.dtype) if with_ctx_out else None
    return yx, yc


def _na_branch(lat, ctx, rpb, with_ctx_out):
    qx, kx, vx = lat
    qc, kc, vc = ctx
    b, S, _ = qx.shape
    dt = qx.dtype
    rows = S // GRID_W
    kh = min(WIN_H, rows)
    n_cb = GRID_W // NA_QB_W
    scale = NA_D ** -0.5

    def heads(t):
        return t.reshape(b, t.shape[1], NA_H, NA_D).transpose(0, 2, 1, 3)

    qg = heads(qx).reshape(b, NA_H, rows, GRID_W, NA_D)
    kg = heads(kx).reshape(b, NA_H, rows, GRID_W, NA_D)
    vg = heads(vx).reshape(b, NA_H, rows, GRID_W, NA_D)
    kch, vch = heads(kc), heads(vc)
    q_cols = np.arange(GRID_W).reshape(n_cb, NA_QB_W)
    win_c0 = np.clip(q_cols - WIN_W // 2, 0, GRID_W - WIN_W)
    k_c0 = np.clip(np.arange(n_cb) * NA_QB_W - WIN_W // 2, 0, GRID_W - NA_KB_W)
    k_cols = k_c0[:, None] + np.arange(NA_KB_W)
    col_ok = (k_cols[:, None, :] >= win_c0[:, :, None]) & (k_cols[:, None, :] < win_c0[:, :, None] + WIN_W)
    col_idx = np.clip(k_cols[:, None, :] - q_cols[:, :, None] + WIN_W - 1, 0, 2 * WIN_W - 2)
    n_loc = kh * NA_KB_W
    mask = np.broadcast_to(col_ok[:, :, None, :], (n_cb, NA_QB_W, kh, NA_KB_W)).reshape(n_cb, NA_QB_W, n_loc)

    def gather_band(t, r0):
        band = lax.dynamic_slice_in_dim(t, r0, kh, axis=2)[:, :, :, k_cols]
        return band.transpose(0, 1, 3, 2, 4, 5).reshape(b, NA_H, n_cb, n_loc, NA_D)

    def row_fn(r):
        r0 = jnp.clip(r - kh // 2, 0, rows - kh)
        k_blk, v_blk = gather_band(kg, r0), gather_band(vg, r0)
        q_r = lax.dynamic_index_in_dim(qg, r, axis=2, keepdims=False).reshape(b, NA_H, n_cb, NA_QB_W, NA_D)
        bias = rpb[:, r0 + jnp.arange(kh) - r + WIN_H - 1][:, :, col_idx]
        bias = bias.transpose(0, 2, 3, 1, 4).reshape(NA_H, n_cb, NA_QB_W, n_loc).astype(F32)
        s_loc = jnp.einsum('bhnqd,bhnkd->bhnqk', q_r, k_blk).astype(F32) * scale + bias
        s_loc = jnp.where(mask, s_loc, NEG_INF)
        s_ctx = jnp.einsum('bhnqd,bhkd->bhnqk', q_r, kch).astype(F32) * scale
        p = jax.nn.softmax(jnp.concatenate([s_loc, s_ctx], -1), axis=-1).astype(dt)
        o = (jnp.einsum('bhnqk,bhnkd->bhnqd', p[..., :n_loc], v_blk)
             + jnp.einsum('bhnqk,bhkd->bhnqd', p[..., n_loc:], vch))
        return o.reshape(b, NA_H, GRID_W, NA_D)

    o = lax.map(row_fn, jnp.arange(rows))
    yx = o.transpose(1, 0, 3, 2, 4).reshape(b, S, NA_H * NA_D)
    yc = None
    if with_ctx_out:
        qch = heads(qc)
        s = jnp.einsum('bhqd,bhkd->bhqk', qch, kch).astype(F32) * scale
        p = jax.nn.softmax(s, axis=-1).astype(dt)
        yc = jnp.einsum('bhqk,bhkd->bhqd', p, vch).transpose(0, 2, 1, 3).reshape(b, qc.shape[1], NA_H * NA_D)
    return yx, yc


def _merge(ya, yb, yc, gate_logits, w_branch, w_out):
    ga, gb, gcg = jnp.split(jax.nn.sigmoid(gate_logits.astype(F32)).astype(ya.dtype), N_BRANCH, axis=-1)
    m = ga * (ya @ w_branch[0]) + gb * (yb @ w_branch[1]) + gcg * (yc @ w_branch[2])
    return m @ w_out


def _token_mixer(hx, hc, w_in, conv_w, a_log, dt_bias, gdn_norm_w, lam_vec, lam_init, diff_norm_w,
                 rpb, w_branch, w_out, cos, sin, with_ctx_out):
    px = _split_cols(hx @ w_in)
    pc = _split_cols(hc @ w_in)
    ya_x, ya_c = _gdn_branch(px[:8], pc[:8], conv_w, a_log, dt_bias, gdn_norm_w, with_ctx_out)
    yb_x, yb_c = _diff_branch(px[8:11], pc[8:11], lam_vec, lam_init, diff_norm_w, cos, sin, with_ctx_out)
    yc_x, yc_c = _na_branch(px[11:14], pc[11:14], rpb, with_ctx_out)
    out_x = _merge(ya_x, yb_x, yc_x, px[14], w_branch, w_out)
    out_c = _merge(ya_c, yb_c, yc_c, pc[14], w_branch, w_out) if with_ctx_out else None
    return out_x, out_c


def _moe(h, w_router, b_router, w1, b1, w2, b2):
    T, D = h.shape
    logits = (h @ w_router).astype(F32) + b_router.astype(F32)
    top_v, top_e = lax.top_k(logits, TOP_K)
    gate = jax.nn.softmax(top_v, axis=-1)
    n_asg = T * TOP_K
    flat_e = top_e.reshape(-1)
    flat_tok = jnp.arange(n_asg, dtype=jnp.int32) // TOP_K
    order = jnp.argsort(flat_e)
    e_sorted = flat_e[order]
    counts = jnp.bincount(flat_e, length=N_EXPERTS)
    padded = (counts + MOE_BLOCK - 1) // MOE_BLOCK * MOE_BLOCK
    pad_end = jnp.cumsum(padded)
    pad_start = pad_end - padded
    start = jnp.cumsum(counts) - counts
    dest = pad_start[e_sorted] + jnp.arange(n_asg) - start[e_sorted]
    n_blk = -(-(n_asg + N_EXPERTS * (MOE_BLOCK - 1)) // MOE_BLOCK)
    n_rows = n_blk * MOE_BLOCK
    row_tok = jnp.full((n_rows,), T, jnp.int32).at[dest].set(flat_tok[order])
    row_gate = jnp.zeros((n_rows,), h.dtype).at[dest].set(gate.reshape(-1)[order].astype(h.dtype))
    blk_e = jnp.minimum(jnp.searchsorted(pad_end, jnp.arange(n_blk) * MOE_BLOCK, side='right'), N_EXPERTS - 1)
    h_pad = jnp.concatenate([h, jnp.zeros((1, D), h.dtype)], 0)

    def expert_block(args):
        rows, e = args
        hu = h_pad[rows] @ w1[e] + b1[e]
        x_glu = jnp.minimum(hu[:, :D_EXPERT], SWIGLU_LIMIT)
        x_lin = jnp.clip(hu[:, D_EXPERT:], -SWIGLU_LIMIT, SWIGLU_LIMIT)
        act = x_glu * jax.nn.sigmoid(SWIGLU_ALPHA * x_glu) * (x_lin + 1)
        return act @ w2[e] + b2[e]

    y = lax.map(expert_block, (row_tok.reshape(n_blk, MOE_BLOCK), blk_e))
    y = y.reshape(n_rows, D) * row_gate[:, None]
    return jax.ops.segment_sum(y, row_tok, num_segments=T + 1)[:T]


def setup_inputs(seed: int = 0) -> dict:
    key = jax.random.key(seed)
    ks = jax.random.split(key, 24)

    def nrm(k, shape, s):
        return jax.random.normal(k, shape, F32) * s

    dt = jnp.exp(jax.random.uniform(ks[9], (DEPTH, 2, GDN_H), F32, math.log(1e-3), math.log(1e-1)))
    return {
        'x': nrm(ks[0], (BATCH, SEQ, D_MODEL), 1.0),
        'c': nrm(ks[1], (BATCH, D_MODEL), 1.0),
        'ctx': nrm(ks[2], (BATCH, CTX_LEN, D_MODEL), 1.0),
        'c_ctx': nrm(ks[3], (D_MODEL,), 1.0),
        'w_ada': nrm(ks[4], (DEPTH, D_MODEL, 6 * D_MODEL), D_MODEL ** -0.5),
        'b_ada': nrm(ks[5], (DEPTH, 6 * D_MODEL), 0.02),
        'w_in': nrm(ks[6], (DEPTH, D_MODEL, D_IN), D_MODEL ** -0.5),
        'conv_w': nrm(ks[7], (DEPTH, CONV_K, 2 * GDN_QK + GDN_V), CONV_K ** -0.5),
        'gdn_a_log': jnp.log(jax.random.uniform(ks[8], (DEPTH, 2, GDN_H), F32, 1.0, 16.0)),
        'gdn_dt_bias': dt + jnp.log(-jnp.expm1(-dt)),
        'gdn_norm_w': 1.0 + nrm(ks[10], (DEPTH, GDN_DV), 0.02),
        'diff_lambda': nrm(ks[11], (DEPTH, 4, DIFF_D), 0.1),
        'diff_norm_w': 1.0 + nrm(ks[12], (DEPTH, 2 * DIFF_D), 0.02),
        'na_rpb': nrm(ks[13], (DEPTH, NA_H, 2 * WIN_H - 1, 2 * WIN_W - 1), 0.02),
        'w_branch': nrm(ks[14], (DEPTH, N_BRANCH, BRANCH_W, D_MODEL), BRANCH_W ** -0.5),
        'w_out': nrm(ks[15], (DEPTH, D_MODEL, D_MODEL), D_MODEL ** -0.5 * DN_BETA),
        'ln_g': 1.0 + nrm(ks[16], (DEPTH, 2, D_MODEL), 0.02),
        'ln_b': nrm(ks[17], (DEPTH, 2, D_MODEL), 0.02),
        'w_router': nrm(ks[18], (DEPTH, D_MODEL, N_EXPERTS), D_MODEL ** -0.5),
        'b_router': nrm(ks[19], (DEPTH, N_EXPERTS), 0.01),
        'w_exp1': nrm(ks[20], (DEPTH, N_EXPERTS, D_MODEL, 2 * D_EXPERT), D_MODEL ** -0.5),
        'b_exp1': nrm(ks[21], (DEPTH, N_EXPERTS, 2 * D_EXPERT), 0.01),
        'w_exp2': nrm(ks[22], (DEPTH, N_EXPERTS, D_EXPERT, D_MODEL), D_EXPERT ** -0.5 * DN_BETA),
        'b_exp2': nrm(ks[23], (DEPTH, N_EXPERTS, D_MODEL), 0.01),
    }


def reference(x, c, ctx, c_ctx, w_ada, b_ada, w_in, conv_w, gdn_a_log, gdn_dt_bias, gdn_norm_w,
              diff_lambda, diff_norm_w, na_rpb, w_branch, w_out, ln_g, ln_b, w_router, b_router,
              w_exp1, b_exp1, w_exp2, b_exp2):
    b, S, D = x.shape
    Lc = ctx.shape[1]
    cos, sin = _axial_rope(S, x.dtype)
    for l in range(DEPTH):
        last = l == DEPTH - 1
        lam_init = 0.8 - 0.6 * math.exp(-0.3 * l)
        mod_x = (jax.nn.silu(c) @ w_ada[l] + b_ada[l])[:, None, :]
        mod_c = jax.nn.silu(c_ctx) @ w_ada[l] + b_ada[l]
        sh1, sc1, g1, sh2, sc2, g2 = jnp.split(mod_x, 6, axis=-1)
        csh1, csc1, cg1, csh2, csc2, cg2 = jnp.split(mod_c, 6, axis=-1)
        hx = _modulate(x, sh1, sc1)
        hc = _modulate(ctx, csh1, csc1)
        mx, mc = _token_mixer(hx, hc, w_in[l], conv_w[l], gdn_a_log[l], gdn_dt_bias[l], gdn_norm_w[l],
                              diff_lambda[l], lam_init, diff_norm_w[l], na_rpb[l], w_branch[l], w_out[l],
                              cos, sin, not last)
        x = _post_norm(x, g1 * mx, ln_g[l, 0], ln_b[l, 0])
        hx = _modulate(x, sh2, sc2)
        if last:
            fx = _moe(hx.reshape(-1, D), w_router[l], b_router[l], w_exp1[l], b_exp1[l],
                      w_exp2[l], b_exp2[l]).reshape(b, S, D)
        else:
            ctx = _post_norm(ctx, cg1 * mc, ln_g[l, 0], ln_b[l, 0])
            hc = _modulate(ctx, csh2, csc2)
            f = _moe(jnp.concatenate([hx.reshape(-1, D), hc.reshape(-1, D)], 0), w_router[l], b_router[l],
                     w_exp1[l], b_exp1[l], w_exp2[l], b_exp2[l])
            fx = f[:b * S].reshape(b, S, D)
            ctx = _post_norm(ctx, cg2 * f[b * S:].reshape(b, Lc, D), ln_g[l, 1], ln_b[l, 1])
        x = _post_norm(x, g2 * fx, ln_g[l, 1], ln_b[l, 1])
    return x
```

```python
import numpy as np
from contextlib import ExitStack
import concourse.bass as bass
import concourse.mybir as mybir
from concourse.bass_utils import run_bass_kernel_spmd

F32 = mybir.dt.float32
BF16 = mybir.dt.bfloat16
ALU = mybir.AluOpType
AF = mybir.ActivationFunctionType
AX = mybir.AxisListType

NDS = 24
STAGES = {"gdn", "diff", "na", "merge", "moe"}
DEBUG_OUT = []
GDN_LEVEL = 99
DUMP_LAST = False
DBG_MOE = None
SAME_SYNC = True
SERIAL = False


_UNIQ = [0]


def _sbt(nc, name, shape, dt):
    _UNIQ[0] += 1
    return nc.sbuf_tensor("%s_u%d" % (name, _UNIQ[0]), shape, dt)


class Buf:
    __slots__ = ("w", "r", "x")

    def __init__(self, x=False):
        self.w = None
        self.r = {}
        self.x = x


class P:
    def __init__(self, nc, es):
        self.nc = nc
        self.es = es
        self.engs = {"pe": nc.tensor, "dve": nc.vector, "act": nc.scalar, "pool": nc.gpsimd, "sp": nc.sync}
        self.ops = {e: [] for e in self.engs}
        self.sem = {e: es.enter_context(nc.semaphore("s_" + e)) for e in self.engs}
        self.cnt = {e: 0 for e in self.engs}
        self.dq = ("sp", "pool")
        self.dsem = {e: [es.enter_context(nc.semaphore("d_%s%d" % (e, i))) for i in range(NDS)] for e in self.dq}
        self.dcnt = {e: [0] * NDS for e in self.dq}
        self.dnext = {e: 0 for e in self.dq}
        self.waited = {e: {} for e in self.engs}
        self.bufs = {}

    def buf(self, key):
        b = self.bufs.get(key)
        if b is None:
            b = self.bufs[key] = Buf()
        return b

    def _deps(self, eng, reads, writes, is_dma):
        evs = []
        for b in reads:
            if b.w is not None:
                evs.append(b.w)
            if b.x:
                evs.extend(b.r.values())
        for b in writes:
            if b.w is not None:
                evs.append(b.w)
            evs.extend(b.r.values())
        wd = self.waited[eng]
        need = {}
        for sem, val, src, isd in evs:
            if (not isd) and src == eng and not is_dma and (eng == "pe" or not SAME_SYNC):
                continue
            k = id(sem)
            if wd.get(k, 0) >= val:
                continue
            if k not in need or need[k][1] < val:
                need[k] = (sem, val)
        out = list(need.values())
        for sem, val in out:
            wd[id(sem)] = val
        return out

    def _commit(self, ev, reads, writes):
        k = id(ev[0])
        for b in reads:
            b.r[k] = ev
        for b in writes:
            b.w = ev
            b.r = {}

    def op(self, eng, fn, reads=(), writes=()):
        waits = self._deps(eng, reads, writes, False)
        if SERIAL and self.__dict__.get("last_ev") is not None:
            sem, val, src, isd = self.last_ev
            if src != eng and self.waited[eng].get(id(sem), 0) < val:
                waits.append((sem, val))
                self.waited[eng][id(sem)] = val
        self.cnt[eng] += 1
        ev = (self.sem[eng], self.cnt[eng], eng, False)
        self.ops[eng].append((waits, fn, self.sem[eng], 1))
        self.last_ev = ev
        self._commit(ev, reads, writes)

    def dma(self, eng, out, in_, reads=(), writes=()):
        k = self.dnext[eng]
        self.dnext[eng] = (k + 1) % NDS
        sem = self.dsem[eng][k]
        waits = self._deps(eng, reads, writes, True)
        prev = self.dcnt[eng][k]
        if prev > 0 and self.waited[eng].get(id(sem), 0) < prev:
            waits.append((sem, prev))
            self.waited[eng][id(sem)] = prev
        self.dcnt[eng][k] += 16
        ev = (sem, self.dcnt[eng][k], eng, True)
        self.ops[eng].append((waits, lambda e: e.dma_start(out=out, in_=in_), sem, 16))
        self._commit(ev, reads, writes)
        return ev

    def idma(self, out, in_, out_off=None, in_off=None, bound=None, reads=(), writes=()):
        eng = "pool"
        k = self.dnext[eng]
        self.dnext[eng] = (k + 1) % NDS
        sem = self.dsem[eng][k]
        waits = self._deps(eng, reads, writes, True)
        prev = self.dcnt[eng][k]
        if prev > 0 and self.waited[eng].get(id(sem), 0) < prev:
            waits.append((sem, prev))
            self.waited[eng][id(sem)] = prev
        self.dcnt[eng][k] += 16
        ev = (sem, self.dcnt[eng][k], eng, True)
        hist = self.__dict__.setdefault("idma_hist", [])
        hist.append(ev)
        if len(hist) > 16:
            osem, oval = hist[-17][0], hist[-17][1]
            if self.waited[eng].get(id(osem), 0) < oval:
                waits.append((osem, oval))
                self.waited[eng][id(osem)] = oval
        oo = bass.IndirectOffsetOnAxis(ap=out_off, axis=0) if out_off is not None else None
        io = bass.IndirectOffsetOnAxis(ap=in_off, axis=0) if in_off is not None else None
        def _f(e):
            try:
                return e.indirect_dma_start(out=out, out_offset=oo, in_=in_, in_offset=io, bounds_check=bound, oob_is_err=False)
            except Exception:
                print("IDMA FAIL", out, in_, out_off, in_off, bound, flush=True)
                raise
        self.ops[eng].append((waits, _f, sem, 16))
        self._commit(ev, reads, writes)

    def barrier(self):
        for e in self.engs:
            waits = []
            wd = self.waited[e]
            for f in self.engs:
                if f != e and self.cnt[f] > 0 and wd.get(id(self.sem[f]), 0) < self.cnt[f]:
                    waits.append((self.sem[f], self.cnt[f]))
                    wd[id(self.sem[f])] = self.cnt[f]
            for q in self.dq:
                for i in range(NDS):
                    v = self.dcnt[q][i]
                    sm = self.dsem[q][i]
                    if v > 0 and wd.get(id(sm), 0) < v:
                        waits.append((sm, v))
                        wd[id(sm)] = v
            self.ops[e].append((waits, None, None, 0))

    def wait_all(self, eng, bufs):
        waits = self._deps(eng, bufs, (), True)
        self.ops[eng].append((waits, None, None, 0))

    def emit(self):
        import os
        if os.environ.get("DUMP_OPS"):
            for name in self.ops:
                print("ENGINE", name, len(self.ops[name]))
                for waits, fn, sem, inc in self.ops[name][-6:]:
                    print("   waits", [(getattr(w[0], "name", str(w[0])), w[1]) for w in waits], "fn", getattr(fn, "__code__", None) and fn.__code__.co_firstlineno, inc)
        with self.nc.Block() as block:
            for name, attr in (("sp", "sync"), ("pe", "tensor"), ("dve", "vector"), ("act", "scalar"), ("pool", "gpsimd")):
                lst = self.ops[name]

                def body(e, lst=lst):
                    for waits, fn, sem, inc in lst:
                        for ws, wv in waits:
                            e.wait_ge(ws, wv)
                        if fn is not None:
                            ins = fn(e)
                            if DUMP_LAST and lst is self.ops["dve"] and len(lst) - lst.index((waits, fn, sem, inc)) <= 4:
                                try:
                                    print("DVEINS", ins.ins if hasattr(ins, "ins") else ins, flush=True)
                                except Exception as ex:
                                    print("DVEINS?", ex)
                            ins.then_inc(sem, inc)

                getattr(block, attr)(body)


class Pool:
    def __init__(self, p, name, n, shape, dtype, psum=False, es=None):
        self.t = []
        es = es or p.es
        for i in range(n):
            if psum:
                t = es.enter_context(p.nc.psum_tensor("%s%d" % (name, i), shape, dtype))
            else:
                t = es.enter_context(_sbt(p.nc, "%s%d" % (name, i), shape, dtype))
            self.t.append((t, Buf(x=psum)))
        self.i = 0

    def get(self):
        r = self.t[self.i]
        self.i = (self.i + 1) % len(self.t)
        return r


D = 1024
KC = 8
LN_EPS = 1e-5
RMS_EPS = 1e-6
DN_ALPHA = 8 ** 0.25


class K:
    def __init__(self, p):
        self.p = p
        nc, es = p.nc, p.es
        self.ps = Pool(p, "ps", 4, [128, 512], F32, psum=True)
        self.psacc = Pool(p, "psacc", 3, [128, 512], F32, psum=True)
        self.psb = Pool(p, "psb", 1, [128, 1024], BF16, psum=True)
        self.identb = es.enter_context(_sbt(nc, "identb", [128, 128], BF16))
        self.identf = es.enter_context(_sbt(nc, "identf", [128, 128], F32))
        self.ones_b = es.enter_context(_sbt(nc, "ones_b", [128, 128], BF16))
        self.ones_f = es.enter_context(_sbt(nc, "ones_f", [128, 128], F32))
        self.cb = Buf()
        self.small = Pool(p, "small", 8, [128, 16], F32)
        self.junk = Pool(p, "junk", 1, [128, D], F32)
        self.stg = Pool(p, "stg", 2, [128, KC, 256], F32)
        self.htl = Pool(p, "htl", 2, [128, KC, 512], BF16)

    def load_consts(self, ident_dram):
        p = self.p
        p.dma("sp", self.identf[:], ident_dram[:, :], writes=[self.cb])
        p.op("dve", lambda e: e.tensor_copy(self.identb[:], self.identf[:]), reads=[self.cb], writes=[self.cb])
        p.op("dve", lambda e: e.memset(self.ones_f[:], 1.0), writes=[self.cb])
        p.op("dve", lambda e: e.memset(self.ones_b[:], 1.0), writes=[self.cb])


def ln_stats(k, xt, xb, width):
    p = k.p
    st, sb = k.small.get()
    junk, jb = k.junk.get()
    p.op("dve", lambda e: e.reduce_sum(st[:, 0:1], xt, axis=AX.X), reads=[xb], writes=[sb])
    p.op("act", lambda e: e.activation(junk[:, 0:width], xt, AF.Square, accum_out=st[:, 1:2]), reads=[xb], writes=[sb, jb])
    p.op("dve", lambda e: e.tensor_scalar(st[:, 2:4], st[:, 0:2], 1.0 / width, None, op0=ALU.mult), reads=[sb], writes=[sb])
    p.op("dve", lambda e: e.tensor_tensor(st[:, 4:5], st[:, 2:3], st[:, 2:3], op=ALU.mult), reads=[sb], writes=[sb])
    p.op("dve", lambda e: e.tensor_tensor(st[:, 5:6], st[:, 3:4], st[:, 4:5], op=ALU.subtract), reads=[sb], writes=[sb])
    p.op("dve", lambda e: e.tensor_scalar(st[:, 5:6], st[:, 5:6], LN_EPS, None, op0=ALU.add), reads=[sb], writes=[sb])
    p.op("act", lambda e: e.activation(st[:, 8:9], st[:, 5:6], AF.Sqrt), reads=[sb], writes=[sb])
    p.op("dve", lambda e: e.reciprocal(st[:, 6:7], st[:, 8:9]), reads=[sb], writes=[sb])
    p.op("dve", lambda e: e.scalar_tensor_tensor(st[:, 7:8], st[:, 2:3], -1.0, st[:, 6:7], op0=ALU.mult, op1=ALU.mult), reads=[sb], writes=[sb])
    return st, sb


def stage_ln_mod(k, X, HT, T, n_lat, modT, modb, sc_chunk, sh_chunk, HTf=None):
    p = k.p
    nt = T // 128
    blk = 4
    for b0 in range(0, nt, blk):
        tiles = list(range(b0, min(b0 + blk, nt)))
        ht, hb = k.htp.get()
        if HTf is not None:
            hf, hfb = k.htfp.get()
        for j, ti in enumerate(tiles):
            which = 0 if ti * 128 < n_lat else 1
            xt, xb = k.xp.get()
            p.dma("sp", xt[:], X[ti * 128:(ti + 1) * 128, :], reads=[p.buf(("X", ti))], writes=[xb])
            st, sb = ln_stats(k, xt[:], xb, D)
            if HTf is None:
                xn, xnb = k.xnp.get()
                p.op("act", lambda e, xn=xn, xt=xt, st=st: e.activation(xn[:], xt[:], AF.Identity, bias=st[:, 7:8], scale=st[:, 6:7]),
                     reads=[xb, sb], writes=[xnb])
                pt, pb = k.psb.get()
                for c in range(KC):
                    p.op("pe", lambda e, pt=pt, xn=xn, c=c: e.transpose(pt[:, c * 128:(c + 1) * 128], xn[:, c * 128:(c + 1) * 128], k.identb[:]),
                         reads=[xnb, k.cb], writes=[pb])
                for c in range(KC):
                    dst = ht[:, c, j * 128:(j + 1) * 128]
                    src = pt[:, c * 128:(c + 1) * 128]
                    s1 = modT[:, sc_chunk + c, which:which + 1]
                    s2 = modT[:, sh_chunk + c, which:which + 1]
                    if c % 2 == 0:
                        p.op("dve", lambda e, dst=dst, src=src, s1=s1, s2=s2: e.tensor_scalar(dst, src, s1, s2, op0=ALU.mult, op1=ALU.add),
                             reads=[pb, modb], writes=[hb])
                    else:
                        p.op("act", lambda e, dst=dst, src=src, s1=s1, s2=s2: e.activation(dst, src, AF.Identity, bias=s2, scale=s1),
                             reads=[pb, modb], writes=[hb])
            else:
                p.op("act", lambda e, xt=xt, st=st: e.activation(xt[:], xt[:], AF.Identity, bias=st[:, 7:8], scale=st[:, 6:7]),
                     reads=[xb, sb], writes=[xb])
                for half in range(2):
                    pt, pb = k.ps.get()
                    for c4 in range(4):
                        c = half * 4 + c4
                        p.op("pe", lambda e, pt=pt, xt=xt, c=c, c4=c4: e.transpose(pt[:, c4 * 128:(c4 + 1) * 128], xt[:, c * 128:(c + 1) * 128], k.identf[:]),
                             reads=[xb, k.cb], writes=[pb])
                    for c4 in range(4):
                        c = half * 4 + c4
                        dst = hf[:, c, j * 128:(j + 1) * 128]
                        src = pt[:, c4 * 128:(c4 + 1) * 128]
                        s1 = modT[:, sc_chunk + c, which:which + 1]
                        s2 = modT[:, sh_chunk + c, which:which + 1]
                        p.op("dve", lambda e, dst=dst, src=src, s1=s1, s2=s2: e.tensor_scalar(dst, src, s1, s2, op0=ALU.mult, op1=ALU.add),
                             reads=[pb, modb], writes=[hfb])
                        dstb = ht[:, c, j * 128:(j + 1) * 128]
                        p.op("act", lambda e, dstb=dstb, dst=dst: e.activation(dstb, dst, AF.Copy), reads=[hfb], writes=[hb])
        t0 = tiles[0] * 128
        w = len(tiles) * 128
        p.dma("pool", HT[:, :, t0:t0 + w].rearrange("c p t -> p c t"), ht[:, :, 0:w], reads=[hb],
              writes=[p.buf(("HT", bb)) for bb in tiles])
        if HTf is not None:
            p.dma("pool", HTf[:, :, t0:t0 + w].rearrange("c p t -> p c t"), hf[:, :, 0:w], reads=[hfb],
                  writes=[p.buf(("HTf", bb)) for bb in tiles])


def mm(k, out, lhsT, rhs, reads, writes, start=True, stop=True):
    k.p.op("pe", lambda e: e.matmul(out, lhsT, rhs, start=start, stop=stop), reads=reads, writes=writes)


def tp(k, out, in_, ident, reads, writes):
    k.p.op("pe", lambda e: e.transpose(out, in_, ident), reads=list(reads) + [k.cb], writes=writes)


NO_POOL = True


def _e(eng):
    return "dve" if (NO_POOL and eng == "pool") else eng


def tt(k, eng, out, a, b, op, reads, writes):
    eng = _e(eng)
    k.p.op(eng, lambda e: e.tensor_tensor(out, a, b, op=op), reads=reads, writes=writes)


def ts(k, eng, out, a, s1, op0, reads, writes, s2=None, op1=None):
    eng = _e(eng)
    if op1 is None:
        k.p.op(eng, lambda e: e.tensor_scalar(out, a, s1, None, op0=op0), reads=reads, writes=writes)
    else:
        k.p.op(eng, lambda e: e.tensor_scalar(out, a, s1, s2, op0=op0, op1=op1), reads=reads, writes=writes)


def stt(k, eng, out, a, scalar, b, op0, op1, reads, writes):
    k.p.op(eng, lambda e: e.scalar_tensor_tensor(out, a, scalar, b, op0=op0, op1=op1), reads=reads, writes=writes)


def af(k, out, in_, func, reads, writes, bias=None, scale=None, accum=None):
    kw = {}
    if bias is not None:
        kw["bias"] = bias
    if scale is not None:
        kw["scale"] = scale
    if accum is not None:
        kw["accum_out"] = accum
    k.p.op("act", lambda e: e.activation(out, in_, func, **kw), reads=reads, writes=writes)


_rr = [0]


def cpy(k, out, in_, reads, writes, psum_src=False):
    _rr[0] += 1
    engs = ("dve", "act") if (psum_src or NO_POOL) else ("dve", "act", "pool")
    eng = engs[_rr[0] % len(engs)]
    if eng == "act":
        af(k, out, in_, AF.Copy, reads, writes)
    else:
        k.p.op(eng, lambda e: e.tensor_copy(out, in_), reads=reads, writes=writes)


def load_w(k, dst, dstb, W, kk, ncols, stg):
    p = k.p
    for c0 in range(0, ncols, 256):
        w = min(256, ncols - c0)
        st, sb = stg.get()
        p.dma("sp", st[:, 0:kk, 0:w], W[:, c0:c0 + w].rearrange("(k p) n -> p k n", p=128), writes=[sb])
        for kc in range(kk):
            cpy(k, dst[:, kc, c0:c0 + w], st[:, kc, 0:w], [sb], [dstb])


def load_ht(k, HT, t0, w):
    ht, hb = k.htl.get()
    k.p.dma("sp", ht[:, :, 0:w], HT[:, :, t0:t0 + w].rearrange("c p t -> p c t"),
            reads=[k.p.buf(("HT", t)) for t in range(t0 // 128, (t0 + w) // 128)], writes=[hb])
    return ht, hb


def blocks(T, n_lat):
    out = []
    t = 0
    while t < T:
        lim = n_lat if t < n_lat else T
        w = min(512, lim - t)
        out.append((t, w))
        t += w
    return out


def stage_mod(k, es, cvec, w_ada_l, b_adaT_l, modT, modb, stg, MODD):
    p = k.p
    cs = es.enter_context(_sbt(p.nc, "cs", [128, KC, 2], F32))
    bt = es.enter_context(_sbt(p.nc, "badaT", [128, 48], F32))
    csb = Buf()
    p.dma("sp", cs[:], cvec[:, :, :], writes=[csb])
    p.dma("sp", bt[:], b_adaT_l[:, :], writes=[csb])
    af(k, cs[:], cs[:], AF.Silu, [csb], [csb])
    pt, pb = k.ps.get()
    for c0 in range(0, 6 * D, 256):
        st, sb = stg.get()
        p.dma("sp", st[:, 0:KC, 0:256], w_ada_l[:, c0:c0 + 256].rearrange("(k p) n -> p k n", p=128), writes=[sb])
        for jj in range(2):
            j = c0 // 128 + jj
            for kc in range(KC):
                mm(k, pt[:, 2 * j:2 * j + 2], st[:, kc, jj * 128:(jj + 1) * 128], cs[:, kc, :], [sb, csb], [pb],
                   start=(kc == 0), stop=(kc == KC - 1))
    pv = pt[:, 0:96].rearrange("p (j w) -> p j w", w=2)
    for w in range(2):
        tt(k, "dve", modT[:, :, w], pv[:, :, w], bt[:, :], ALU.add, [pb, csb], [modb])
    for c0 in (8, 32):
        ts(k, "dve", modT[:, c0:c0 + 8, :], modT[:, c0:c0 + 8, :], 1.0, ALU.add, [modb], [modb])
    for w in range(2):
        pt2, pb2 = k.ps.get()
        tp(k, pt2[0:48, 0:128], modT[:, :, w], k.identf[:], [modb], [pb2])
        rt = es.enter_context(_sbt(p.nc, "modrow%d" % w, [48, 128], F32))
        rtb = Buf()
        cpy(k, rt[:], pt2[0:48, 0:128], [pb2], [rtb], psum_src=True)
        p.dma("sp", MODD[w, :].rearrange("(j p) -> j p", p=128), rt[:], reads=[rtb], writes=[p.buf("MODD")])


GDN_OFF = dict(q=0, k=512, v=1024, z=1536, ab=2048)


def stage_gdn(k, es0, HT, T, n_lat, w_in_l, conv_wT_l, alog_rep_l, dtb_rep_l, gnorm_l, gmask, OF, YAT, stg, tmask_d):
    p, nc = k.p, k.p.nc
    NT = T // 128
    NTL = n_lat // 128
    blks = blocks(T, n_lat)
    with ExitStack() as es:
        sb_ = lambda name, shape, dt: es.enter_context(_sbt(nc, name, shape, dt))
        G = sb_("g_G", [128, NT, 16], F32)
        Bt = sb_("g_B", [128, NT, 16], F32)
        crep = sb_("g_crep", [128, 2, NT * 16], F32)
        gb = Buf()
        wab = sb_("g_wab", [128, KC, 16], BF16)
        wabb = Buf()
        masks = sb_("g_masks", [128, 6, 128], F32)
        mb = Buf()
        p.dma("sp", masks[:], gmask.rearrange("m p f -> p m f"), writes=[mb])
        tmk = sb_("g_tmk", [128, 2, 7, 128], F32)
        p.dma("sp", tmk[:], tmask_d.rearrange("a l p f -> p a l f"), writes=[mb])
        offp = Pool(p, "g_off", 4, [128, 7, 128], BF16, es=es)
        p.dma("sp", crep[:, 0, :], alog_rep_l[:, :], writes=[gb])
        p.dma("sp", crep[:, 1, :], dtb_rep_l[:, :], writes=[gb])
        load_w(k, wab, wabb, w_in_l[:, 2048:2064], KC, 16, stg)
        for (t0, w) in blks:
            ht, hb = load_ht(k, HT, t0, w)
            pt, pb = k.ps.get()
            nj = w // 128
            for j in range(nj):
                for kc in range(KC):
                    mm(k, pt[:, j * 16:(j + 1) * 16], ht[:, kc, j * 128:(j + 1) * 128], wab[:, kc, :], [hb, wabb], [pb],
                       start=(kc == 0), stop=(kc == KC - 1))
            ti = t0 // 128
            cpy(k, G[:, ti:ti + nj, :], pt[:, 0:nj * 16].rearrange("p (j c) -> p j c", c=16), [pb], [gb], psum_src=True)
        Gf = G[:, :, :].rearrange("p j c -> p (j c)")
        Bf = Bt[:, :, :].rearrange("p j c -> p (j c)")
        Tf = crep[:, 1, :]
        af(k, Bf, Gf, AF.Sigmoid, [gb], [gb])
        tt(k, "dve", Tf, Gf, crep[:, 1, :], ALU.add, [gb], [gb])
        af(k, Tf, Tf, AF.Exp, [gb], [gb])
        af(k, Tf, Tf, AF.Ln, [gb], [gb], bias=1.0)
        af(k, crep[:, 0, :], crep[:, 0, :], AF.Exp, [gb], [gb])
        stt(k, "dve", Gf, Tf, -1.0, crep[:, 0, :], ALU.mult, ALU.mult, [gb], [gb])

        if GDN_LEVEL <= 1:
            p.barrier()
            return
        raw = sb_("g_raw", [128, T + 4], BF16)
        rawb = Buf()
        qF = sb_("g_qF", [128, T], BF16)
        kF = sb_("g_kF", [128, T], BF16)
        vF = sb_("g_vF", [128, T], BF16)
        zs = sb_("g_zs", [128, T], BF16)
        fb = {"q": Buf(), "k": Buf(), "v": Buf(), "z": Buf()}
        wh = sb_("g_wh", [128, KC, 512], BF16)
        whb = Buf()
        cw = sb_("g_cw", [128, 3, 3], F32)
        nw = sb_("g_nw", [128, 1], F32)
        cwb = Buf()
        p.dma("sp", nw[:], gnorm_l[:, :], writes=[cwb])
        S = sb_("g_S", [128, 128], F32)
        Sb = sb_("g_Sb", [128, 128], BF16)
        Sbuf = Buf()
        f32p = Pool(p, "g_f", 10, [128, 128], F32, es=es)
        b16p = Pool(p, "g_h", 14, [128, 128], BF16, es=es)
        b16l = Pool(p, "g_hl", 16, [128, 128], BF16, es=es)
        wide = Pool(p, "g_w", 4, [128, 512], F32, es=es)
        wideb = Pool(p, "g_wb", 2, [128, 512], BF16, es=es)
        obp = Pool(p, "g_ob", 2, [128, 512], F32, es=es)
        p.op("dve", lambda e: e.memset(raw[:], 0.0), writes=[rawb])
        for h in range(4):
            for wi, nm in enumerate(("q", "k", "v", "z")):
                load_w(k, wh[:, :, wi * 128:(wi + 1) * 128], whb,
                       w_in_l[:, GDN_OFF[nm] + h * 128:GDN_OFF[nm] + (h + 1) * 128], KC, 128, stg)
            p.dma("sp", cw[:], conv_wT_l[:, h * 128:(h + 1) * 128, :].rearrange("w c k -> c w k"), writes=[cwb])
            for wi, nm in enumerate(("z", "q", "k", "v")):
                widx = ("q", "k", "v", "z").index(nm)
                for (t0, w) in blks:
                    ht, hb = load_ht(k, HT, t0, w)
                    pt, pb = k.ps.get()
                    for kc in range(KC):
                        mm(k, pt[:, 0:w], wh[:, kc, widx * 128:(widx + 1) * 128], ht[:, kc, 0:w], [hb, whb], [pb],
                           start=(kc == 0), stop=(kc == KC - 1))
                    if nm == "z":
                        af(k, zs[:, t0:t0 + w], pt[:, 0:w], AF.Silu, [pb], [fb["z"]])
                    else:
                        off = 1 + t0 if t0 < n_lat else 3 + t0
                        cpy(k, raw[:, off:off + w], pt[:, 0:w], [pb], [rawb], psum_src=True)
                if nm == "z":
                    continue
                for (t0, w) in blks:
                    off = 1 + t0 if t0 < n_lat else 3 + t0
                    tm, tb = wide.get()
                    ts(k, "dve", tm[:, 0:w], raw[:, off:off + w], cw[:, widx, 1:2], ALU.mult, [rawb, cwb], [tb])
                    stt(k, "dve", tm[:, 0:w], raw[:, off - 1:off - 1 + w], cw[:, widx, 0:1], tm[:, 0:w], ALU.mult, ALU.add, [rawb, cwb, tb], [tb])
                    stt(k, "dve", tm[:, 0:w], raw[:, off + 1:off + 1 + w], cw[:, widx, 2:3], tm[:, 0:w], ALU.mult, ALU.add, [rawb, cwb, tb], [tb])
                    if nm == "v":
                        af(k, vF[:, t0:t0 + w], tm[:, 0:w], AF.Silu, [tb], [fb["v"]])
                        continue
                    af(k, tm[:, 0:w], tm[:, 0:w], AF.Silu, [tb], [tb])
                    sq, sqb = wideb.get()
                    tt(k, "pool", sq[:, 0:w], tm[:, 0:w], tm[:, 0:w], ALU.mult, [tb], [sqb])
                    pt, pb = k.ps.get()
                    mm(k, pt[:, 0:w], k.ones_b[:], sq[:, 0:w], [sqb, k.cb], [pb])
                    rs, rb = wide.get()
                    ts(k, "dve", rs[:, 0:w], pt[:, 0:w], RMS_EPS, ALU.add, [pb], [rb])
                    af(k, rs[:, 0:w], rs[:, 0:w], AF.Sqrt, [rb], [rb])
                    p.op("dve", lambda e, rs=rs, w=w: e.reciprocal(rs[:, 0:w], rs[:, 0:w]), reads=[rb], writes=[rb])
                    dst = qF if nm == "q" else kF
                    scl = 128 ** -0.5 if nm == "q" else 1.0
                    stt(k, "dve", dst[:, t0:t0 + w], tm[:, 0:w], scl, rs[:, 0:w], ALU.mult, ALU.mult, [tb, rb], [fb[nm]])
            if GDN_LEVEL <= 2:
                p.barrier()
                return
            for d in range(2):
                MI, NEG, STR = masks[:, d * 3 + 0, :], masks[:, d * 3 + 1, :], masks[:, d * 3 + 2, :]
                colg = (0 if d == 0 else 8) + h
                colb = (4 if d == 0 else 12) + h
                p.op("dve", lambda e: e.memset(S[:], 0.0), writes=[Sbuf])
                p.op("dve", lambda e: e.memset(Sb[:], 0.0), writes=[Sbuf])
                order = list(range(NTL, NT)) + list(range(NTL)) if d == 0 else list(range(NT - 1, NTL - 1, -1)) + list(range(NTL - 1, -1, -1))
                groups = []
                for ti in order:
                    g0 = (ti // 4) * 4
                    if groups and groups[-1][0] == g0:
                        groups[-1][1].append(ti)
                    else:
                        groups.append((g0, [ti]))
                for g0, tis in groups:
                    gw = len(tis) * 128
                    ob, obb = obp.get()
                    for ti in tis:
                        c = slice(ti * 128, (ti + 1) * 128)
                        jo = (ti - g0) * 128
                        gcol = G[:, ti, colg:colg + 1]
                        bcol = Bt[:, ti, colb:colb + 1]
                        Mg, Mgb = f32p.get()
                        ts(k, "dve", Mg[:], MI, gcol, ALU.mult, [mb, gb], [Mgb])
                        psA, pAb = k.ps.get()
                        mm(k, psA[:, 0:128], k.ones_f[:], Mg[:], [Mgb, k.cb], [pAb])
                        mm(k, psA[:, 128:129], MI, gcol, [mb, gb], [pAb])
                        mm(k, psA[:, 129:130], k.ones_f[:], gcol, [k.cb, gb], [pAb])
                        sc, scb = k.small.get()
                        ts(k, "dve", sc[:, 0:1], psA[:, 128:129], -1.0, ALU.mult, [pAb], [scb])
                        cpy(k, sc[:, 3:4], psA[:, 129:130], [pAb], [scb], psum_src=True)
                        af(k, sc[:, 1:2], psA[:, 128:129], AF.Exp, [pAb], [scb])
                        af(k, sc[:, 2:3], psA[:, 128:129], AF.Exp, [pAb, scb], [scb], bias=sc[:, 3:4], scale=-1.0)
                        af(k, sc[:, 4:5], sc[:, 3:4], AF.Exp, [scb], [scb])
                        tt(k, "dve", sc[:, 5:6], sc[:, 1:2], bcol, ALU.mult, [scb, gb], [scb])
                        if GDN_LEVEL <= 3:
                            p.barrier()
                            return
                        DT, DTb = f32p.get()
                        tt(k, "dve", DT[:], psA[:, 0:128], NEG, ALU.add, [pAb, mb], [DTb])
                        af(k, DT[:], DT[:], AF.Exp, [DTb, scb], [DTb], bias=sc[:, 0:1])
                        eB, eBb = f32p.get()
                        af(k, eB[:], psA[:, 0:128], AF.Exp, [pAb], [eBb])
                        if GDN_LEVEL <= 3.2:
                            p.barrier()
                            return
                        Ib, Ibb = f32p.get()
                        ts(k, "pool", Ib[:], k.identf[:], bcol, ALU.mult, [k.cb, gb], [Ibb])
                        psB, pBb = k.ps.get()
                        mm(k, psB[:, 0:128], k.ones_f[:], Ib[:], [Ibb, k.cb], [pBb])
                        mm(k, psB[:, 128:256], kF[:, c], kF[:, c], [fb["k"]], [pBb])
                        mm(k, psB[:, 256:384], kF[:, c], qF[:, c], [fb["k"], fb["q"]], [pBb])
                        if GDN_LEVEL <= 3.4:
                            p.barrier()
                            return
                        DTs, DTsb = f32p.get()
                        tt(k, "pool", DTs[:], DT[:], STR, ALU.mult, [DTb, mb], [DTsb])
                        U, Ub_ = f32p.get()
                        tt(k, "dve", U[:], psB[:, 128:256], DTs[:], ALU.mult, [pBb, DTsb], [Ub_])
                        tt(k, "dve", U[:], psB[:, 0:128], U[:], ALU.mult, [pBb, Ub_], [Ub_])
                        Pm, Pmb = b16p.get()
                        cpy(k, Pm[:], U[:], [Ub_], [Pmb])
                        qkT, qkb = b16l.get()
                        tt(k, "dve", qkT[:], psB[:, 256:384], DT[:], ALU.mult, [pBb, DTb], [qkb])
                        if GDN_LEVEL <= 3.6:
                            p.barrier()
                            return
                        ptb, ptbb = k.psb.get()
                        tp(k, ptb[:, 0:128], Pm[:], k.identb[:], [Pmb], [ptbb])
                        tp(k, ptb[:, 128:256], vF[:, c], k.identb[:], [fb["v"]], [ptbb])
                        tp(k, ptb[:, 256:384], kF[:, c], k.identb[:], [fb["k"]], [ptbb])
                        if GDN_LEVEL <= 3.8:
                            p.barrier()
                            return
                        Am, Amb = b16p.get()
                        cpy(k, Am[:], ptb[:, 0:128], [ptbb], [Amb], psum_src=True)
                        if GDN_LEVEL <= 3.85:
                            p.barrier()
                            return
                        vb, vbb = b16l.get()
                        import os as _os
                        _v = _os.environ.get("VBV", "")
                        if _v == "1":
                            k.p.op("dve", lambda e, vb=vb, ptb=ptb: e.tensor_copy(vb[:], ptb[:, 128:256]), reads=[ptbb], writes=[vbb])
                        elif _v == "2":
                            ts(k, "dve", vb[:], ptb[:, 0:128], bcol, ALU.mult, [ptbb, gb], [vbb])
                        elif _v == "3":
                            ts(k, "dve", vb[:], ptb[:, 128:256], 0.5, ALU.mult, [ptbb, gb], [vbb])
                        else:
                            af(k, vb[:], ptb[:, 128:256], AF.Copy, [ptbb, gb], [vbb], scale=bcol)
                        if GDN_LEVEL <= 3.9:
                            p.barrier()
                            return
                        kbg, kbgb = b16l.get()
                        af(k, kbg[:], ptb[:, 256:384], AF.Copy, [ptbb, scb], [kbgb], scale=sc[:, 5:6])
                        if GDN_LEVEL <= 3.95:
                            p.barrier()
                            return
                        ktl, ktlb = b16l.get()
                        af(k, ktl[:], ptb[:, 256:384], AF.Copy, [ptbb, scb], [ktlb], scale=sc[:, 2:3])
                        if GDN_LEVEL <= 4:
                            p.barrier()
                            return
                        mu = tmk[:, d, :, :]
                        ml = tmk[:, 1 - d, :, :]
                        UO, UOb = offp.get()
                        AO, AOb = offp.get()
                        tt(k, "dve", UO[:], Pm[:].unsqueeze(1).to_broadcast([128, 7, 128]), mu, ALU.mult, [Pmb, mb], [UOb])
                        tt(k, "dve", AO[:], Am[:].unsqueeze(1).to_broadcast([128, 7, 128]), ml, ALU.mult, [Amb, mb], [AOb])
                        R, Rb = b16p.get()
                        Tm, Tmb = b16p.get()
                        tt(k, "dve", R[:], k.identb[:], UO[:, 0, :], ALU.subtract, [k.cb, UOb], [Rb])
                        tt(k, "dve", Tm[:], k.identb[:], AO[:, 0, :], ALU.subtract, [k.cb, AOb], [Tmb])
                        for lv in range(1, 7):
                            last = lv == 6
                            psN, pNb = k.ps.get()
                            mm(k, psN[:, 0:128], AO[:, lv, :], R[:], [AOb, Rb], [pNb])
                            if not last:
                                mm(k, psN[:, 128:256], UO[:, lv, :], Tm[:], [UOb, Tmb], [pNb])
                            X, Xb = b16p.get()
                            cpy(k, X[:], psN[:, 0:128], [pNb], [Xb], psum_src=True)
                            if not last:
                                X2, X2b = b16p.get()
                                cpy(k, X2[:], psN[:, 128:256], [pNb], [X2b], psum_src=True)
                            mm(k, psN[:, 256:384], Tm[:], X[:], [Tmb, Xb], [pNb])
                            if not last:
                                mm(k, psN[:, 384:512], R[:], X2[:], [Rb, X2b], [pNb])
                            R2, R2b = b16p.get()
                            tt(k, "dve", R2[:], R[:], psN[:, 256:384], ALU.subtract, [pNb, Rb], [R2b])
                            if not last:
                                T2, T2b = b16p.get()
                                tt(k, "dve", T2[:], Tm[:], psN[:, 384:512], ALU.subtract, [pNb, Tmb], [T2b])
                                Tm, Tmb = T2, T2b
                            R, Rb = R2, R2b
                        if GDN_LEVEL <= 5:
                            p.barrier()
                            return
                        psC, pCb = k.ps.get()
                        mm(k, psC[:, 0:128], R[:], vb[:], [Rb, vbb], [pCb])
                        mm(k, psC[:, 128:256], kbg[:], R[:], [Rb, kbgb], [pCb])
                        usb, usbb = f32p.get()
                        cpy(k, usb[:], psC[:, 0:128], [pCb], [usbb], psum_src=True)
                        wT, wTb = b16l.get()
                        cpy(k, wT[:], psC[:, 128:256], [pCb], [wTb], psum_src=True)
                        if GDN_LEVEL <= 5.2:
                            p.barrier()
                            return
                        qg, qgb = b16l.get()
                        tt(k, "pool", qg[:], qF[:, c], eB[:], ALU.mult, [fb["q"], eBb], [qgb])
                        psD, pDb = k.ps.get()
                        mm(k, psD[:, 0:128], wT[:], Sb[:], [wTb, Sbuf], [pDb])
                        if GDN_LEVEL <= 5.4:
                            p.barrier()
                            return
                        vn, vnb = b16l.get()
                        tt(k, "dve", vn[:], usb[:], psD[:, 0:128], ALU.subtract, [usbb, pDb], [vnb])
                        mm(k, psD[:, 128:256], Sb[:], qg[:], [Sbuf, qgb], [pDb], start=True, stop=False)
                        mm(k, psD[:, 128:256], vn[:], qkT[:], [vnb, qkb], [pDb], start=False, stop=True)
                        mm(k, psD[:, 256:384], ktl[:], vn[:], [ktlb, vnb], [pDb])
                        if GDN_LEVEL <= 5.6:
                            p.barrier()
                            return
                        cpy(k, ob[:, jo:jo + 128], psD[:, 128:256], [pDb], [obb], psum_src=True)
                        if GDN_LEVEL <= 5.8:
                            p.barrier()
                            return
                        stt(k, "dve", S[:], S[:], sc[:, 4:5], psD[:, 256:384], ALU.mult, ALU.add, [Sbuf, scb, pDb], [Sbuf])
                        af(k, Sb[:], S[:], AF.Copy, [Sbuf], [Sbuf])
                    t0 = g0 * 128
                    ofk = [p.buf(("OF", h, t)) for t in tis]
                    if d == 0:
                        p.dma("pool", OF[h, :, t0:t0 + gw], ob[:, 0:gw], reads=[obb], writes=ofk)
                    else:
                        of, ofb = wide.get()
                        p.dma("sp", of[:, 0:gw], OF[h, :, t0:t0 + gw], reads=ofk, writes=[ofb])
                        tt(k, "dve", ob[:, 0:gw], ob[:, 0:gw], of[:, 0:gw], ALU.add, [obb, ofb], [obb])
                        sq, sqb = wideb.get()
                        tt(k, "pool", sq[:, 0:gw], ob[:, 0:gw], ob[:, 0:gw], ALU.mult, [obb], [sqb])
                        pt, pb = k.ps.get()
                        mm(k, pt[:, 0:gw], k.ones_b[:], sq[:, 0:gw], [sqb, k.cb], [pb])
                        rs, rb = wide.get()
                        ts(k, "dve", rs[:, 0:gw], pt[:, 0:gw], 1.0 / 128, ALU.mult, [pb], [rb], s2=RMS_EPS, op1=ALU.add)
                        af(k, rs[:, 0:gw], rs[:, 0:gw], AF.Sqrt, [rb], [rb])
                        p.op("dve", lambda e, rs=rs, gw=gw: e.reciprocal(rs[:, 0:gw], rs[:, 0:gw]), reads=[rb], writes=[rb])
                        stt(k, "dve", ob[:, 0:gw], ob[:, 0:gw], nw[:, 0:1], rs[:, 0:gw], ALU.mult, ALU.mult, [obb, rb, cwb], [obb])
                        yo, yob = wideb.get()
                        tt(k, "dve", yo[:, 0:gw], ob[:, 0:gw], zs[:, t0:t0 + gw], ALU.mult, [obb, fb["z"]], [yob])
                        p.dma("pool", YAT[h * 128:(h + 1) * 128, t0:t0 + gw], yo[:, 0:gw], reads=[yob],
                              writes=[p.buf(("YA", h, t)) for t in tis])
    p.barrier()


def stage_diff(k, HT, T, n_lat, w_in_l, w_perm_l, lam_l, lam_init, dnorm_l, cosT, sinT, YBT, stg):
    p, nc = k.p, k.p.nc
    NT = T // 128
    NTL = n_lat // 128
    blks = blocks(T, n_lat)
    QO, KO, VO = 2064, 2064 + 512, 2064 + 1024
    with ExitStack() as es:
        sb_ = lambda name, shape, dt: es.enter_context(_sbt(nc, name, shape, dt))
        qT = sb_("d_qT", [128, T], BF16)
        kT = sb_("d_kT", [128, T], BF16)
        V = sb_("d_V", [128, NT, 128], BF16)
        fb = {"q": Buf(), "k": Buf(), "v": Buf()}
        wh = sb_("d_wh", [128, KC, 5 * 128], BF16)
        whb = Buf()
        nw = sb_("d_nw", [128, 1], F32)
        lam = sb_("d_lam", [128, 4], F32)
        lv = sb_("d_lv", [1, 256], F32)
        lb = Buf()
        wide = Pool(p, "d_w", 6, [128, 512], F32, es=es)
        pbp = Pool(p, "d_p", 4, [128, 512], BF16, es=es)
        ybp = Pool(p, "d_y", 2, [128, 512], BF16, es=es)
        p.dma("sp", lv[:], lam_l[:, :], writes=[lb])
        p.dma("sp", nw[:], dnorm_l[:, :], writes=[lb])
        p.op("dve", lambda e: e.memset(lam[:], 0.0), writes=[lb])
        tt(k, "dve", lv[0:1, 0:64], lv[0:1, 0:64], lv[0:1, 64:128], ALU.mult, [lb], [lb])
        tt(k, "dve", lv[0:1, 128:192], lv[0:1, 128:192], lv[0:1, 192:256], ALU.mult, [lb], [lb])
        p.op("dve", lambda e: e.reduce_sum(lam[0:1, 0:1], lv[0:1, 0:64], axis=AX.X), reads=[lb], writes=[lb])
        p.op("dve", lambda e: e.reduce_sum(lam[0:1, 1:2], lv[0:1, 128:192], axis=AX.X), reads=[lb], writes=[lb])
        af(k, lam[0:1, 0:2], lam[0:1, 0:2], AF.Exp, [lb], [lb])
        tt(k, "dve", lam[0:1, 2:3], lam[0:1, 1:2], lam[0:1, 0:1], ALU.subtract, [lb], [lb])
        ts(k, "dve", lam[0:1, 2:3], lam[0:1, 2:3], -lam_init, ALU.add, [lb], [lb])
        pt, pb = k.ps.get()
        mm(k, pt[:, 0:1], k.ones_f[0:1, :], lam[0:1, 2:3], [lb, k.cb], [pb])
        cpy(k, lam[:, 3:4], pt[:, 0:1], [pb], [lb], psum_src=True)
        nlam = lam[:, 3:4]
        ts(k, "dve", nw[:], nw[:], 1.0 - lam_init, ALU.mult, [lb], [lb])
        for h in range(4):
            load_w(k, wh[:, :, 0:128], whb, w_in_l[:, QO + h * 128:QO + (h + 1) * 128], KC, 128, stg)
            load_w(k, wh[:, :, 128:256], whb, w_in_l[:, KO + h * 128:KO + (h + 1) * 128], KC, 128, stg)
            load_w(k, wh[:, :, 256:384], whb, w_in_l[:, VO + h * 128:VO + (h + 1) * 128], KC, 128, stg)
            load_w(k, wh[:, :, 384:512], whb, w_perm_l[:, h * 128:(h + 1) * 128], KC, 128, stg)
            load_w(k, wh[:, :, 512:640], whb, w_perm_l[:, 512 + h * 128:512 + (h + 1) * 128], KC, 128, stg)
            for (t0, w) in blks:
                ht, hb = load_ht(k, HT, t0, w)
                lat = t0 < n_lat
                if lat:
                    cs, csb = wide.get()
                    sn, snb = wide.get()
                    p.dma("pool", cs[:, 0:w], cosT[:, t0:t0 + w], writes=[csb])
                    p.dma("pool", sn[:, 0:w], sinT[:, t0:t0 + w], writes=[snb])
                for wi, (dst, nm, scl) in enumerate(((qT, "q", 0.125), (kT, "k", 1.0))):
                    pt, pb = k.ps.get()
                    for kc in range(KC):
                        mm(k, pt[:, 0:w], wh[:, kc, wi * 128:(wi + 1) * 128], ht[:, kc, 0:w], [hb, whb], [pb],
                           start=(kc == 0), stop=(kc == KC - 1))
                    if not lat:
                        ts(k, "dve", dst[:, t0:t0 + w], pt[:, 0:w], scl, ALU.mult, [pb], [fb[nm]])
                        continue
                    pt2, pb2 = k.ps.get()
                    for kc in range(KC):
                        mm(k, pt2[:, 0:w], wh[:, kc, 384 + wi * 128:384 + (wi + 1) * 128], ht[:, kc, 0:w], [hb, whb], [pb2],
                           start=(kc == 0), stop=(kc == KC - 1))
                    a, ab = wide.get()
                    tt(k, "dve", a[:, 0:w], pt[:, 0:w], cs[:, 0:w], ALU.mult, [pb, csb], [ab])
                    b2, bb2 = wide.get()
                    tt(k, "dve", b2[:, 0:w], pt2[:, 0:w], sn[:, 0:w], ALU.mult, [pb2, snb], [bb2])
                    if scl != 1.0:
                        ts(k, "pool", b2[:, 0:w], b2[:, 0:w], scl, ALU.mult, [bb2], [bb2])
                    stt(k, "dve", dst[:, t0:t0 + w], a[:, 0:w], scl, b2[:, 0:w], ALU.mult, ALU.add, [ab, bb2], [fb[nm]])
                pt, pb = k.ps.get()
                nj = w // 128
                for j in range(nj):
                    for kc in range(KC):
                        mm(k, pt[:, j * 128:(j + 1) * 128], ht[:, kc, j * 128:(j + 1) * 128], wh[:, kc, 256:384], [hb, whb], [pb],
                           start=(kc == 0), stop=(kc == KC - 1))
                ti = t0 // 128
                cpy(k, V[:, ti:ti + nj, :], pt[:, 0:nj * 128].rearrange("p (j c) -> p j c", c=128), [pb], [fb["v"]], psum_src=True)
            for (t0, w) in blks:
                keys = list(range(NT)) if t0 < n_lat else list(range(NTL, NT))
                res = []
                for m in range(2):
                    ms = slice(m * 64, (m + 1) * 64)
                    po, pob = k.psacc.get()
                    pl, plb = k.psacc.get()
                    for ki, kt in enumerate(keys):
                        pss, psb_ = k.ps.get()
                        mm(k, pss[:, 0:w], kT[ms, kt * 128:(kt + 1) * 128], qT[ms, t0:t0 + w], [fb["k"], fb["q"]], [psb_])
                        pe_, peb = pbp.get()
                        af(k, pe_[:, 0:w], pss[:, 0:w], AF.Exp, [psb_], [peb])
                        st_, sp_ = (ki == 0), (ki == len(keys) - 1)
                        mm(k, po[:, 0:w], V[:, kt, :], pe_[:, 0:w], [fb["v"], peb], [pob], start=st_, stop=sp_)
                        mm(k, pl[:, 0:w], k.ones_b[:], pe_[:, 0:w], [k.cb, peb], [plb], start=st_, stop=sp_)
                    rl, rlb = wide.get()
                    p.op("dve", lambda e, rl=rl, pl=pl, w=w: e.reciprocal(rl[:, 0:w], pl[:, 0:w]), reads=[plb], writes=[rlb])
                    o, ob = wide.get()
                    tt(k, "dve", o[:, 0:w], po[:, 0:w], rl[:, 0:w], ALU.mult, [pob, rlb], [ob])
                    res.append((o, ob))
                (o1, o1b), (o2, o2b) = res
                stt(k, "dve", o1[:, 0:w], o2[:, 0:w], nlam, o1[:, 0:w], ALU.mult, ALU.add, [o1b, o2b, lb], [o1b])
                sq, sqb = pbp.get()
                tt(k, "pool", sq[:, 0:w], o1[:, 0:w], o1[:, 0:w], ALU.mult, [o1b], [sqb])
                pt, pb = k.ps.get()
                mm(k, pt[:, 0:w], k.ones_b[:], sq[:, 0:w], [sqb, k.cb], [pb])
                rs, rb = wide.get()
                ts(k, "dve", rs[:, 0:w], pt[:, 0:w], 1.0 / 128, ALU.mult, [pb], [rb], s2=RMS_EPS, op1=ALU.add)
                af(k, rs[:, 0:w], rs[:, 0:w], AF.Sqrt, [rb], [rb])
                p.op("dve", lambda e, rs=rs, w=w: e.reciprocal(rs[:, 0:w], rs[:, 0:w]), reads=[rb], writes=[rb])
                yo, yob = ybp.get()
                stt(k, "dve", yo[:, 0:w], o1[:, 0:w], nw[:, 0:1], rs[:, 0:w], ALU.mult, ALU.mult, [o1b, rb, lb], [yob])
                p.dma("pool", YBT[h * 128:(h + 1) * 128, t0:t0 + w], yo[:, 0:w], reads=[yob],
                      writes=[p.buf(("YB", h, t)) for t in range(t0 // 128, (t0 + w) // 128)])
    p.barrier()


GRID_W = 64
NA_KC0 = (0, 8, 24, 32)


def stage_na(k, HT, T, n_lat, w_in_l, bias_l, rowmask, VN, YCT, stg):
    p, nc = k.p, k.p.nc
    NT = T // 128
    NTL = n_lat // 128
    n_ctx = T - n_lat
    NCT = n_ctx // 128
    rows = n_lat // GRID_W
    NM = rows // 8
    blks = blocks(T, n_lat)
    QO = 2064 + 1536
    KO, VO = QO + 512, QO + 1024
    with ExitStack() as es:
        sb_ = lambda name, shape, dt: es.enter_context(_sbt(nc, name, shape, dt))
        qT = sb_("n_qT", [128, T], BF16)
        kS = sb_("n_kS", [128, 4, (n_lat // GRID_W) * 32], BF16)
        kC = sb_("n_kC", [128, max(n_ctx, 128)], BF16)
        yrow = sb_("n_y", [128, T], BF16)
        ypl = Pool(p, "n_ypl", 2, [128, 512], BF16, es=es)
        fb = {"q": Buf(), "k": Buf(), "y": Buf()}
        wh = sb_("n_wh", [128, KC, 512], BF16)
        whb = Buf()
        rmk = sb_("n_rm", [128, 3, 512], F32)
        rmb = Buf()
        p.dma("sp", rmk[:], rowmask.rearrange("c p f -> p c f"), writes=[rmb])
        btm = Pool(p, "n_bt", 4, [128, 3, 512], F32, es=es)
        braw = Pool(p, "n_br", 2, [128, 512], F32, es=es)
        vtp = Pool(p, "n_vt", 4, [128, 4, 128], BF16, es=es)
        vcx = sb_("n_vc", [128, max(NCT, 1), 128], BF16)
        vcb = Buf()
        ssb = Pool(p, "n_s", 3, [128, 512], F32, es=es)
        pbp = Pool(p, "n_p", 3, [128, 768], BF16, es=es)
        rlp = Pool(p, "n_rl", 3, [128, 128], F32, es=es)
        for c0 in range(0, 512, 512):
            load_w(k, wh, whb, w_in_l[:, VO:VO + 512], KC, 512, stg)
        vst = Pool(p, "n_vs", 2, [128, 512], BF16, es=es)
        for (t0, w) in blks:
            ht, hb = load_ht(k, HT, t0, w)
            for j in range(w // 128):
                pt, pb = k.ps.get()
                for kc in range(KC):
                    mm(k, pt[:, :], ht[:, kc, j * 128:(j + 1) * 128], wh[:, kc, :], [hb, whb], [pb], start=(kc == 0), stop=(kc == KC - 1))
                vs, vsb = vst.get()
                cpy(k, vs[:], pt[:, :], [pb], [vsb], psum_src=True)
                ti = t0 // 128 + j
                p.dma("pool", VN[ti * 128:(ti + 1) * 128, :], vs[:], reads=[vsb], writes=[p.buf(("VN", ti))])
        vn_all = [p.buf(("VN", ti)) for ti in range(NT)]
        for g in range(4):
            load_w(k, wh[:, :, 0:128], whb, w_in_l[:, QO + g * 128:QO + (g + 1) * 128], KC, 128, stg)
            load_w(k, wh[:, :, 128:256], whb, w_in_l[:, KO + g * 128:KO + (g + 1) * 128], KC, 128, stg)
            for (t0, w) in blks:
                ht, hb = load_ht(k, HT, t0, w)
                for wi, nm in enumerate(("q", "k")):
                    pt, pb = k.ps.get()
                    for kc in range(KC):
                        mm(k, pt[:, 0:w], wh[:, kc, wi * 128:(wi + 1) * 128], ht[:, kc, 0:w], [hb, whb], [pb],
                           start=(kc == 0), stop=(kc == KC - 1))
                    if t0 >= n_lat:
                        if nm == "q":
                            ts(k, "dve", qT[:, t0:t0 + w], pt[:, 0:w], 0.125, ALU.mult, [pb], [fb["q"]])
                        else:
                            cpy(k, kC[:, t0 - n_lat:t0 - n_lat + w], pt[:, 0:w], [pb], [fb["k"]], psum_src=True)
                    elif nm == "q":
                        ts(k, "dve", qT[:, t0:t0 + 512].rearrange("p (n qr qc) -> p qr n qc", n=4, qr=8, qc=16),
                           pt[:, 0:512].rearrange("p (qr n qc) -> p qr n qc", n=4, qr=8, qc=16), 0.125, ALU.mult, [pb], [fb["q"]])
                    else:
                        mrow = t0 // 512
                        for n in range(4):
                            cpy(k, kS[:, n, mrow * 256:(mrow + 1) * 256].rearrange("p (r c) -> p r c", c=32),
                                pt[:, 0:512].rearrange("p (r c) -> p r c", c=64)[:, :, NA_KC0[n]:NA_KC0[n] + 32], [pb], [fb["k"]], psum_src=True)
            for j in range(NCT):
                p.dma("sp", vcx[:, j, :], VN[n_lat + j * 128:n_lat + (j + 1) * 128, g * 128:(g + 1) * 128], reads=vn_all, writes=[vcb])
            for n in range(4):
                bts = []
                for hh in range(2):
                    br, brb = braw.get()
                    p.dma("sp", br[:], bias_l[g * 2 + hh, n, :, :], writes=[brb])
                    bt, btb = btm.get()
                    for cs_ in range(3):
                        tt(k, "pool", bt[:, cs_, :], br[:], rmk[:, cs_, :], ALU.add, [brb, rmb], [btb])
                    bts.append((bt, btb))
                for m in range(NM):
                    case = 0 if m == 0 else (2 if m == NM - 1 else 1)
                    vt, vtb = vtp.get()
                    for t in range(4):
                        r0 = min(max(8 * m - 4 + 4 * t, 0), rows - 4)
                        for r_ in range(4):
                            tok = (r0 + r_) * 64 + NA_KC0[n]
                            p.dma("sp" if r_ % 2 else "pool", vt[r_ * 32:(r_ + 1) * 32, t, :], VN[tok:tok + 32, g * 128:(g + 1) * 128], reads=vn_all, writes=[vtb])
                    for hh in range(2):
                        hs = slice(hh * 64, (hh + 1) * 64)
                        bt, btb = bts[hh]
                        qap = qT[hs, m * 512 + n * 128:m * 512 + (n + 1) * 128]
                        psl, pslb = k.ps.get()
                        for t in range(4):
                            r0 = min(max(8 * m - 4 + 4 * t, 0), rows - 4)
                            kap = kS[hs, n, r0 * 32:r0 * 32 + 128]
                            mm(k, psl[:, t * 128:(t + 1) * 128], kap, qap, [fb["k"], fb["q"]], [pslb])
                        psc, pscb = k.ps.get()
                        for j in range(NCT):
                            mm(k, psc[:, j * 128:(j + 1) * 128], kC[hs, j * 128:(j + 1) * 128], qap, [fb["k"], fb["q"]], [pscb])
                        s_, s_b = ssb.get()
                        tt(k, "dve", s_[:], psl[:], bt[:, case, :], ALU.add, [pslb, btb], [s_b])
                        pe_, peb = pbp.get()
                        af(k, pe_[:, 0:512], s_[:], AF.Exp, [s_b], [peb])
                        af(k, pe_[:, 512:512 + NCT * 128], psc[:, 0:NCT * 128], AF.Exp, [pscb], [peb])
                        po, pob = k.ps.get()
                        nk = 4 + NCT
                        for t in range(nk):
                            vap = vt[:, t, :] if t < 4 else vcx[:, t - 4, :]
                            rd = [vtb, peb] if t < 4 else [vcb, peb]
                            mm(k, po[:, 0:128], vap, pe_[:, t * 128:(t + 1) * 128], rd, [pob], start=(t == 0), stop=(t == nk - 1))
                        for t in range(nk):
                            mm(k, po[:, 128:256], k.ones_b[:, :], pe_[:, t * 128:(t + 1) * 128], [k.cb, peb], [pob], start=(t == 0), stop=(t == nk - 1))
                        rl, rlb = rlp.get()
                        p.op("dve", lambda e, rl=rl, po=po, hs=hs: e.reciprocal(rl[hs, :], po[hs, 128:256]), reads=[pob], writes=[rlb])
                        tt(k, "dve", yrow[hs, m * 512 + n * 128:m * 512 + (n + 1) * 128], po[hs, 0:128], rl[hs, :], ALU.mult, [pob, rlb], [fb["y"]])
            for hh in range(2):
                hs = slice(hh * 64, (hh + 1) * 64)
                for jq in range(NCT):
                    qap = qT[hs, n_lat + jq * 128:n_lat + (jq + 1) * 128]
                    psc, pscb = k.ps.get()
                    for j in range(NCT):
                        mm(k, psc[:, j * 128:(j + 1) * 128], kC[hs, j * 128:(j + 1) * 128], qap, [fb["k"], fb["q"]], [pscb])
                    pe_, peb = pbp.get()
                    af(k, pe_[:, 0:NCT * 128], psc[:, 0:NCT * 128], AF.Exp, [pscb], [peb])
                    po, pob = k.ps.get()
                    for t in range(NCT):
                        mm(k, po[:, 0:128], vcx[:, t, :], pe_[:, t * 128:(t + 1) * 128], [vcb, peb], [pob], start=(t == 0), stop=(t == NCT - 1))
                    for t in range(NCT):
                        mm(k, po[:, 128:256], k.ones_b[:, :], pe_[:, t * 128:(t + 1) * 128], [k.cb, peb], [pob], start=(t == 0), stop=(t == NCT - 1))
                    rl, rlb = rlp.get()
                    p.op("dve", lambda e, rl=rl, po=po, hs=hs: e.reciprocal(rl[hs, :], po[hs, 128:256]), reads=[pob], writes=[rlb])
                    tt(k, "dve", yrow[hs, n_lat + jq * 128:n_lat + (jq + 1) * 128], po[hs, 0:128], rl[hs, :], ALU.mult, [pob, rlb], [fb["y"]])
            for m in range(NM):
                yp_, ypb = ypl.get()
                cpy(k, yp_[:, :].rearrange("p (qr n qc) -> p qr n qc", n=4, qr=8, qc=16),
                    yrow[:, m * 512:(m + 1) * 512].rearrange("p (n qr qc) -> p qr n qc", n=4, qr=8, qc=16), [fb["y"]], [ypb])
                p.dma("pool", YCT[g * 128:(g + 1) * 128, m * 512:(m + 1) * 512], yp_[:, :], reads=[ypb], writes=[p.buf(("YC", g))])
            p.dma("pool", YCT[g * 128:(g + 1) * 128, n_lat:T], yrow[:, n_lat:T], reads=[fb["y"]], writes=[p.buf(("YC", g))])
    p.barrier()


def post_norm_tile(k, xt, xb, yt, yb, gateB, gB, bB, cb_, out_eng_alt):
    p = k.p
    tt(k, "pool", yt, yt, gateB, ALU.mult, [yb, cb_], [yb])
    stt(k, "dve", xt, xt, DN_ALPHA, yt, ALU.mult, ALU.add, [xb, yb], [xb])
    st, sb = ln_stats(k, xt, xb, D)
    af(k, xt, xt, AF.Identity, [xb, sb], [xb], bias=st[:, 7:8], scale=st[:, 6:7])
    tt(k, "dve", xt, xt, gB, ALU.mult, [xb, cb_], [xb])
    tt(k, "pool", xt, xt, bB, ALU.add, [xb, cb_], [xb])


def load_rows(k, es, name, srcs):
    out = []
    b = Buf()
    for i, src in enumerate(srcs):
        t = es.enter_context(_sbt(k.p.nc, "%s%d" % (name, i), [128, D], F32))
        k.p.dma("sp", t[:], src.partition_broadcast(128), reads=[k.p.buf("MODD")], writes=[b])
        out.append(t)
    return out, b


def stage_merge(k, HT, T, n_lat, w_in_l, w_branch_l, w_out_l, YT, X, g1rows, lng, lnb, stg):
    p, nc = k.p, k.p.nc
    blks = blocks(T, n_lat)
    GO = 5136
    with ExitStack() as es:
        sb_ = lambda name, shape, dt: es.enter_context(_sbt(nc, name, shape, dt))
        wg = sb_("m_wg", [128, KC, 3 * D], BF16)
        wbr = sb_("m_wbr", [128, 3, 4, D], BF16)
        wo = sb_("m_wo", [128, KC, D], BF16)
        wb_ = Buf()
        load_w(k, wg, wb_, w_in_l[:, GO:GO + 3 * D], KC, 3 * D, stg)
        for br in range(3):
            load_w(k, wbr[:, br, :, :], wb_, w_branch_l[br, :, :], 4, D, stg)
        load_w(k, wo, wb_, w_out_l[:, :], KC, D, stg)
        rows, rb = load_rows(k, es, "m_r", [g1rows[0], g1rows[1], lng, lnb])
        yin = Pool(p, "m_y", 1, [128, 3, 4, 512], BF16, es=es)
        mT = Pool(p, "m_m", 1, [128, KC, 512], BF16, es=es)
        sgp = Pool(p, "m_sg", 3, [128, 512], F32, es=es)
        acc = Pool(p, "m_acc", 2, [128, 512], F32, es=es)
        xp = Pool(p, "m_x", 1, [128, D], F32, es=es)
        yp = Pool(p, "m_yo", 1, [128, D], F32, es=es)
        for (t0, w) in blks:
            which = 0 if t0 < n_lat else 1
            ht, hb = load_ht(k, HT, t0, w)
            yi, yib = yin.get()
            tl = list(range(t0 // 128, (t0 + w) // 128))
            for br in range(3):
                rd = [p.buf((("YA", "YB")[br], h, t)) for h in range(4) for t in tl] if br < 2 else [p.buf(("YC", g)) for g in range(4)]
                p.dma("pool", yi[:, br, :, 0:w], YT[br][:, t0:t0 + w].rearrange("(c p) t -> p c t", p=128), reads=rd, writes=[yib])
            m_, mb_ = mT.get()
            for j in range(KC):
                ac, acb = acc.get()
                for br in range(3):
                    pg, pgb = k.ps.get()
                    for kc in range(KC):
                        mm(k, pg[:, 0:w], wg[:, kc, br * D + j * 128:br * D + (j + 1) * 128], ht[:, kc, 0:w], [wb_, hb], [pgb],
                           start=(kc == 0), stop=(kc == KC - 1))
                    py, pyb = k.ps.get()
                    for kc in range(4):
                        mm(k, py[:, 0:w], wbr[:, br, kc, j * 128:(j + 1) * 128], yi[:, br, kc, 0:w], [wb_, yib], [pyb],
                           start=(kc == 0), stop=(kc == 3))
                    sg, sgb = sgp.get()
                    af(k, sg[:, 0:w], pg[:, 0:w], AF.Sigmoid, [pgb], [sgb])
                    if br == 0:
                        tt(k, "dve", ac[:, 0:w], sg[:, 0:w], py[:, 0:w], ALU.mult, [sgb, pyb], [acb])
                    else:
                        tt(k, "dve", sg[:, 0:w], sg[:, 0:w], py[:, 0:w], ALU.mult, [sgb, pyb], [sgb])
                        if br == 1:
                            tt(k, "pool", ac[:, 0:w], ac[:, 0:w], sg[:, 0:w], ALU.add, [acb, sgb], [acb])
                        else:
                            tt(k, "pool", m_[:, j, 0:w], ac[:, 0:w], sg[:, 0:w], ALU.add, [acb, sgb], [mb_])
            for jt in range(w // 128):
                ti = t0 // 128 + jt
                xt, xb = xp.get()
                p.dma("sp", xt[:], X[ti * 128:(ti + 1) * 128, :], reads=[p.buf(("X", ti))], writes=[xb])
                yt, yb = yp.get()
                for half in range(2):
                    po, pob = k.ps.get()
                    for kc in range(KC):
                        mm(k, po[:, :], m_[:, kc, jt * 128:(jt + 1) * 128], wo[:, kc, half * 512:(half + 1) * 512], [mb_, wb_], [pob],
                           start=(kc == 0), stop=(kc == KC - 1))
                    cpy(k, yt[:, half * 512:(half + 1) * 512], po[:, :], [pob], [yb], psum_src=True)
                post_norm_tile(k, xt[:], xb, yt[:], yb, rows[which][:], rows[2][:], rows[3][:], rb, 0)
                p.dma("pool", X[ti * 128:(ti + 1) * 128, :], xt[:], reads=[xb], writes=[p.buf(("X", ti))])
    p.barrier()


U32 = mybir.dt.uint32
SWIGLU_LIMIT = 7.0
SWIGLU_ALPHA = 1.702


def stage_moe(k, X, T, n_lat, MODD, lng, lnb, w_router_l, b_router_l, w1_l, b1T_l, w2_l, b2_l, HT2):
    p, nc = k.p, k.p.nc
    NT = T // 128
    NTL = n_lat // 128
    NE = 32
    SBT = 12
    with ExitStack() as es:
        sb_ = lambda name, shape, dt: es.enter_context(_sbt(nc, name, shape, dt))
        GATES = sb_("e_gates", [128, NT, NE], F32)
        gbuf = Buf()
        wr = sb_("e_wr", [128, KC, NE], F32)
        brr = sb_("e_brr", [128, NE], F32)
        cb_ = Buf()
        p.dma("sp", wr[:], w_router_l.rearrange("(k p) n -> p k n", p=128), writes=[cb_])
        p.dma("sp", brr[:], b_router_l.partition_broadcast(128), writes=[cb_])
        with ExitStack() as esA:
            rows, rb = load_rows(k, esA, "e_r", [MODD[0, 4 * D:5 * D], MODD[1, 4 * D:5 * D], MODD[0, 3 * D:4 * D], MODD[1, 3 * D:4 * D]])
            xp = Pool(p, "e_x", 2, [128, D], F32, es=esA)
            htf = Pool(p, "e_htf", 2, [128, KC, 128], F32, es=esA)
            htb = Pool(p, "e_htb", 2, [128, KC, 128], BF16, es=esA)
            sm = Pool(p, "e_sm", 3, [128, 4 * NE], F32, es=esA)
            m8 = Pool(p, "e_m8", 2, [128, 16], F32, es=esA)
            for ti in range(NT):
                which = 0 if ti < NTL else 1
                xt, xb = xp.get()
                p.dma("sp", xt[:], X[ti * 128:(ti + 1) * 128, :], reads=[p.buf(("X", ti))], writes=[xb])
                st, sb = ln_stats(k, xt[:], xb, D)
                af(k, xt[:], xt[:], AF.Identity, [xb, sb], [xb], bias=st[:, 7:8], scale=st[:, 6:7])
                tt(k, "dve", xt[:], xt[:], rows[which][:], ALU.mult, [xb, rb], [xb])
                tt(k, "pool", xt[:], xt[:], rows[2 + which][:], ALU.add, [xb, rb], [xb])
                hf, hfb = htf.get()
                hb_, hbb = htb.get()
                for half in range(2):
                    pt, pb = k.ps.get()
                    for c4 in range(4):
                        c = half * 4 + c4
                        tp(k, pt[:, c4 * 128:(c4 + 1) * 128], xt[:, c * 128:(c + 1) * 128], k.identf[:], [xb], [pb])
                    k.p.op("dve", lambda e, hf=hf, pt=pt, half=half: e.tensor_copy(hf[:, half * 4:(half + 1) * 4, :], pt[:, :].rearrange("p (c t) -> p c t", t=128)),
                           reads=[pb], writes=[hfb])
                    af(k, hb_[:, half * 4:(half + 1) * 4, :], pt[:, :].rearrange("p (c t) -> p c t", t=128), AF.Copy, [pb], [hbb])
                p.dma("pool", HT2[:, :, ti * 128:(ti + 1) * 128].rearrange("c p t -> p c t"), hb_[:], reads=[hbb], writes=[p.buf(("HT2", ti))])
                pl, plb = k.ps.get()
                for kc in range(KC):
                    mm(k, pl[:, 0:NE], hf[:, kc, :], wr[:, kc, :], [hfb, cb_], [plb], start=(kc == 0), stop=(kc == KC - 1))
                s_, s_b = sm.get()
                lg, sel, ex = (s_[:, i * NE:(i + 1) * NE] for i in range(3))
                tt(k, "dve", lg, pl[:, 0:NE], brr[:], ALU.add, [plb, cb_], [s_b])
                mx, mxb = m8.get()
                p.op("dve", lambda e, mx=mx, lg=lg: e.max(out=mx[:, 0:8], in_=lg), reads=[s_b], writes=[mxb])
                ts(k, "dve", sel, lg, mx[:, 3:4], ALU.is_ge, [s_b, mxb], [s_b])
                ts(k, "dve", mx[:, 8:9], mx[:, 0:1], -1.0, ALU.mult, [mxb], [mxb])
                af(k, ex, lg, AF.Exp, [s_b, mxb], [s_b], bias=mx[:, 8:9])
                tt(k, "dve", ex, ex, sel, ALU.mult, [s_b], [s_b])
                p.op("dve", lambda e, mx=mx, ex=ex: e.reduce_sum(mx[:, 9:10], ex, axis=AX.X), reads=[s_b], writes=[mxb])
                p.op("dve", lambda e, mx=mx: e.reciprocal(mx[:, 9:10], mx[:, 9:10]), reads=[mxb], writes=[mxb])
                ts(k, "dve", GATES[:, ti, :], ex, mx[:, 9:10], ALU.mult, [s_b, mxb], [gbuf])
        p.barrier()
        with ExitStack() as esB:
            w1b = esB.enter_context(_sbt(nc, "e_w1", [128, KC, 2 * D], BF16))
            w2b = esB.enter_context(_sbt(nc, "e_w2", [128, KC, D], BF16))
            wbuf = Buf()
            acc = esB.enter_context(_sbt(nc, "e_acc", [128, SBT, D], F32))
            accb = Buf()
            aTp = Pool(p, "e_aT", 2, [128, KC, 512], BF16, es=esB)
            b1 = Pool(p, "e_b1", 2, [128, 16], F32, es=esB)
            b2 = Pool(p, "e_b2", 2, [128, D], F32, es=esB)
            tmpf = Pool(p, "e_tf", 6, [128, 512], F32, es=esB)
            for s0 in range(0, NT, SBT):
                tiles = list(range(s0, min(s0 + SBT, NT)))
                sblks = [tiles[i:i + 4] for i in range(0, len(tiles), 4)]
                p.op("dve", lambda e: e.memset(acc[:], 0.0), writes=[accb])
                for e_ in range(NE):
                    load_w(k, w1b, wbuf, w1_l[e_, :, :], KC, 2 * D, k.stg)
                    load_w(k, w2b, wbuf, w2_l[e_, :, :], KC, D, k.stg)
                    b1t, b1b = b1.get()
                    p.dma("sp", b1t[:], b1T_l[e_, :, :], writes=[b1b])
                    b2t, b2b = b2.get()
                    p.dma("sp", b2t[:], b2_l[e_, :].partition_broadcast(128), writes=[b2b])
                    for bt in sblks:
                        t0, w = bt[0] * 128, len(bt) * 128
                        ht, hb = k.htl.get()
                        p.dma("sp", ht[:, :, 0:w], HT2[:, :, t0:t0 + w].rearrange("c p t -> p c t"), reads=[p.buf(("HT2", t)) for t in bt], writes=[hb])
                        aT, aTb = aTp.get()
                        for j in range(KC):
                            pg, pgb = k.ps.get()
                            pl, plb = k.ps.get()
                            for kc in range(KC):
                                mm(k, pg[:, 0:w], w1b[:, kc, j * 128:(j + 1) * 128], ht[:, kc, 0:w], [wbuf, hb], [pgb], start=(kc == 0), stop=(kc == KC - 1))
                            for kc in range(KC):
                                mm(k, pl[:, 0:w], w1b[:, kc, D + j * 128:D + (j + 1) * 128], ht[:, kc, 0:w], [wbuf, hb], [plb], start=(kc == 0), stop=(kc == KC - 1))
                            g, gb_ = tmpf.get()
                            ts(k, "dve", g[:, 0:w], pg[:, 0:w], b1t[:, j:j + 1], ALU.add, [pgb, b1b], [gb_], s2=SWIGLU_LIMIT, op1=ALU.min)
                            sg, sgb = tmpf.get()
                            af(k, sg[:, 0:w], g[:, 0:w], AF.Sigmoid, [gb_], [sgb], scale=SWIGLU_ALPHA)
                            l_, lb_ = tmpf.get()
                            ts(k, "dve", l_[:, 0:w], pl[:, 0:w], b1t[:, 8 + j:9 + j], ALU.add, [plb, b1b], [lb_], s2=SWIGLU_LIMIT, op1=ALU.min)
                            ts(k, "pool", l_[:, 0:w], l_[:, 0:w], -SWIGLU_LIMIT, ALU.max, [lb_], [lb_], s2=1.0, op1=ALU.add)
                            tt(k, "pool", g[:, 0:w], g[:, 0:w], sg[:, 0:w], ALU.mult, [gb_, sgb], [gb_])
                            tt(k, "dve", aT[:, j, 0:w], g[:, 0:w], l_[:, 0:w], ALU.mult, [gb_, lb_], [aTb])
                        for jt, ti in enumerate(bt):
                            ai = ti - s0
                            for half in range(2):
                                po, pob = k.ps.get()
                                for j in range(KC):
                                    mm(k, po[:, :], aT[:, j, jt * 128:(jt + 1) * 128], w2b[:, j, half * 512:(half + 1) * 512], [aTb, wbuf], [pob], start=(j == 0), stop=(j == KC - 1))
                                y_, yb_ = tmpf.get()
                                tt(k, "dve", y_[:], po[:, :], b2t[:, half * 512:(half + 1) * 512], ALU.add, [pob, b2b], [yb_])
                                stt(k, "dve", acc[:, ai, half * 512:(half + 1) * 512], y_[:], GATES[:, ti, e_:e_ + 1], acc[:, ai, half * 512:(half + 1) * 512],
                                    ALU.mult, ALU.add, [yb_, gbuf, accb], [accb])
                with ExitStack() as esC:
                    rows4, rb = load_rows(k, esC, "e_rc%d" % s0, [MODD[0, 5 * D:6 * D], MODD[1, 5 * D:6 * D], lng, lnb])
                    xpc = Pool(p, "e_xc%d" % s0, 2, [128, D], F32, es=esC)
                    for ti in tiles:
                        which = 0 if ti < NTL else 1
                        xt, xb = xpc.get()
                        p.dma("sp", xt[:], X[ti * 128:(ti + 1) * 128, :], reads=[p.buf(("X", ti))], writes=[xb])
                        post_norm_tile(k, xt[:], xb, acc[:, ti - s0, :], accb, rows4[which][:], rows4[2][:], rows4[3][:], rb, 0)
                        p.dma("pool", X[ti * 128:(ti + 1) * 128, :], xt[:], reads=[xb], writes=[p.buf(("X", ti))])
                    p.barrier()
    p.barrier()


def build(n_lat, n_ctx, L, lam_inits, cap):
    T = n_lat + n_ctx
    NT = T // 128
    nc = bass.Bass("TRN2", target_bir_lowering=False)

    def din(name, shape, dt=F32):
        return nc.dram_tensor(name, list(shape), dt, kind="ExternalInput").ap()

    x0 = din("x0", [T, D])
    cvec = din("cvec", [128, KC, 2])
    w_ada = din("w_ada", [L, D, 6 * D])
    b_adaT = din("b_adaT", [L, 128, 48])
    w_in = din("w_in", [L, D, 8208])
    w_perm = din("w_perm", [L, D, 1024])
    conv_wT = din("conv_wT", [L, 3, 512, 3])
    alog_rep = din("alog_rep", [L, 128, NT * 16])
    dtb_rep = din("dtb_rep", [L, 128, NT * 16])
    gnorm = din("gnorm", [L, 128, 1])
    lamv = din("lamv", [L, 1, 256])
    dnorm = din("dnorm", [L, 128, 1])
    na_bias = din("na_bias", [L, 8, 4, 128, 512])
    rowmask = din("rowmask", [3, 128, 512])
    w_branch = din("w_branch", [L, 3, 512, D])
    w_out = din("w_out", [L, D, D])
    ln_g = din("ln_g", [L, 2, D])
    ln_b = din("ln_b", [L, 2, D])
    w_router = din("w_router", [L, D, 32])
    b_router = din("b_router", [L, 32])
    w1 = din("w_exp1", [L, 32, D, 2 * D])
    b1T = din("b1T", [L, 32, 128, 16])
    w2 = din("w_exp2", [L, 32, D, D])
    b2 = din("b_exp2", [L, 32, D])
    ident = din("ident", [128, 128])
    gmask = din("gmask", [6, 128, 128])
    tmask_d = din("tmask", [2, 7, 128, 128])
    cosT = din("cosT", [128, n_lat])
    sinT = din("sinT", [128, n_lat])
    iota_d = din("iota", [128, 32])
    sut_d = din("sut", [128, 128])

    X = nc.dram_tensor("out", [T, D], F32, kind="ExternalOutput").ap()
    kd = lambda n: dict(kind="ExternalOutput") if n in DEBUG_OUT else {}
    HT = nc.dram_tensor("HT", [KC, 128, T], BF16, **kd("HT")).ap()
    OF = nc.dram_tensor("OF", [4, 128, T], F32, **kd("OF")).ap()
    YAT = nc.dram_tensor("YAT", [512, T], BF16, **kd("YAT")).ap()
    YBT = nc.dram_tensor("YBT", [512, T], BF16, **kd("YBT")).ap()
    YCT = nc.dram_tensor("YCT", [512, T], BF16, **kd("YCT")).ap()
    VN = nc.dram_tensor("VN", [T, 512], BF16).ap()
    HT2 = nc.dram_tensor("HT2", [KC, 128, T], BF16).ap()
    MODD = nc.dram_tensor("MODD", [2, 6 * D], F32).ap()

    global DBG_MOE
    if "DSTD" in DEBUG_OUT:
        DBG_MOE = (nc.dram_tensor("DSTD", [128, NT, 4], U32, kind="ExternalOutput").ap(),
                   nc.dram_tensor("GATED", [128, NT, 4], F32, kind="ExternalOutput").ap())
    with ExitStack() as es:
        p = P(nc, es)
        k = K(p)
        k.load_consts(ident)
        for ti in range(NT):
            p.dma("sp" if ti % 2 else "pool", X[ti * 128:(ti + 1) * 128, :], x0[ti * 128:(ti + 1) * 128, :], writes=[p.buf(("X", ti))])
        for l in range(L):
            with ExitStack() as esl:
                modT = esl.enter_context(_sbt(nc, "modT", [128, 48, 2], F32))
                modb = Buf()
                stage_mod(k, esl, cvec, w_ada[l], b_adaT[l], modT, modb, k.stg, MODD)
                k.htp = Pool(p, "lnht", 2, [128, KC, 512], BF16, es=esl)
                k.xp = Pool(p, "lnx", 3, [128, D], F32, es=esl)
                k.xnp = Pool(p, "lnxn", 2, [128, D], BF16, es=esl)
                stage_ln_mod(k, X, HT, T, n_lat, modT, modb, 8, 0)
                p.barrier()
            if "gdn" in STAGES:
              stage_gdn(k, None, HT, T, n_lat, w_in[l], conv_wT[l], alog_rep[l], dtb_rep[l], gnorm[l], gmask, OF, YAT, k.stg, tmask_d)
            if "diff" in STAGES:
              stage_diff(k, HT, T, n_lat, w_in[l], w_perm[l], lamv[l], lam_inits[l], dnorm[l], cosT, sinT, YBT, k.stg)
            if "na" in STAGES:
              stage_na(k, HT, T, n_lat, w_in[l], na_bias[l], rowmask, VN, YCT, k.stg)
            if "merge" in STAGES:
              stage_merge(k, HT, T, n_lat, w_in[l], w_branch[l], w_out[l], (YAT, YBT, YCT), X,
                        (MODD[0, 2 * D:3 * D], MODD[1, 2 * D:3 * D]), ln_g[l, 0, :], ln_b[l, 0, :], k.stg)
            if "moe" in STAGES:
              stage_moe(k, X, T, n_lat, MODD, ln_g[l, 1, :], ln_b[l, 1, :], w_router[l], b_router[l], w1[l], b1T[l], w2[l], b2[l], HT2)
        p.wait_all("sp", [p.buf(("X", ti)) for ti in range(NT)])
        p.barrier()
        p.emit()
    return nc


WIN_H, WIN_W = 8, 16
NEG = -1e30


def _rope_tables(n_lat):
    t = np.arange(n_lat)
    row = (t // GRID_W).astype(np.float32)
    col = (t % GRID_W).astype(np.float32)
    inv = (np.float32(10000.0) ** (-np.arange(16, dtype=np.float32) / np.float32(16))).astype(np.float32)
    ang = np.concatenate([row[:, None] * inv, col[:, None] * inv], -1).astype(np.float32)
    ang = np.concatenate([ang, ang], -1)
    cos = np.cos(ang).astype(np.float32).T
    sin = np.sin(ang).astype(np.float32).T
    sgn = np.where(np.arange(64) < 32, -1.0, 1.0).astype(np.float32)[:, None]
    sinS = sin * sgn
    return np.ascontiguousarray(np.concatenate([cos, cos], 0)), np.ascontiguousarray(np.concatenate([sinS, sinS], 0))


def _na_tables(rpb_l, n_lat):
    rows = n_lat // GRID_W
    krl = np.arange(16)
    kc = np.arange(32)
    qr = np.arange(8)
    qc = np.arange(16)
    bias = np.empty((8, 4, 16, 32, 8, 16), np.float32)
    for n in range(4):
        kc_abs = NA_KC0[n] + kc
        qc_abs = 16 * n + qc
        dc = kc_abs[:, None] - qc_abs[None, :]
        cidx = np.clip(dc + WIN_W - 1, 0, 2 * WIN_W - 2)
        wc0 = np.clip(qc_abs - WIN_W // 2, 0, GRID_W - WIN_W)
        col_ok = (kc_abs[:, None] >= wc0[None, :]) & (kc_abs[:, None] < wc0[None, :] + WIN_W)
        dr = (krl[:, None] - 4) - qr[None, :]
        ridx = np.clip(dr + WIN_H - 1, 0, 2 * WIN_H - 2)
        g = rpb_l[:, ridx[:, None, :, None], cidx[None, :, None, :]]
        bias[:, n] = np.where(col_ok[None, None, :, None, :], g, np.float32(NEG))
    b = bias.reshape(8, 4, 4, 4, 32, 8, 16).transpose(0, 1, 3, 4, 2, 5, 6).reshape(8, 4, 128, 512)
    NM = rows // 8
    rm = np.empty((3, 16, 32, 8, 16), np.float32)
    for case, m in enumerate((0, 1, NM - 1)):
        krow = 8 * m - 4 + krl
        r = 8 * m + qr
        r0 = np.clip(r - WIN_H // 2, 0, rows - WIN_H)
        ok = (krow[:, None] >= r0[None, :]) & (krow[:, None] < r0[None, :] + WIN_H) & (krow[:, None] >= 0) & (krow[:, None] < rows)
        rm[case] = np.where(ok[:, None, :, None], np.float32(0), np.float32(NEG))
    rm = rm.reshape(3, 4, 4, 32, 8, 16).transpose(0, 2, 3, 1, 4, 5).reshape(3, 128, 512)
    return np.ascontiguousarray(b), np.ascontiguousarray(rm)


def prep(inp, b, n_lat, n_ctx, L):
    T = n_lat + n_ctx
    NT = T // 128
    f = lambda a: np.ascontiguousarray(np.asarray(a, dtype=np.float32))
    m = {}
    m["x0"] = f(np.concatenate([inp["x"][b, :n_lat], inp["ctx"][b, :n_ctx]], 0))
    cv = np.stack([np.asarray(inp["c"][b]), np.asarray(inp["c_ctx"])], -1).reshape(KC, 128, 2).transpose(1, 0, 2)
    m["cvec"] = f(cv)
    m["w_ada"] = f(inp["w_ada"][:L])
    m["b_adaT"] = f(np.asarray(inp["b_ada"][:L]).reshape(L, 48, 128).transpose(0, 2, 1))
    w_in = np.asarray(inp["w_in"][:L])
    m["w_in"] = f(w_in)
    idx = (np.arange(1024) // 64) * 64 + ((np.arange(1024) % 64) + 32) % 64
    m["w_perm"] = f(np.concatenate([w_in[:, :, 2064:2064 + 512][:, :, idx[:512]], w_in[:, :, 2064 + 512:2064 + 1024][:, :, idx[:512]]], -1))
    m["conv_wT"] = f(np.asarray(inp["conv_w"][:L]).transpose(0, 2, 1).reshape(L, 3, 512, 3))
    al = np.zeros((L, 16), np.float32)
    db = np.zeros((L, 16), np.float32)
    a_log = np.asarray(inp["gdn_a_log"][:L])
    dtb = np.asarray(inp["gdn_dt_bias"][:L])
    al[:, 0:4], al[:, 8:12] = a_log[:, 0], a_log[:, 1]
    db[:, 0:4], db[:, 8:12] = dtb[:, 0], dtb[:, 1]
    m["alog_rep"] = f(np.broadcast_to(np.tile(al, (1, NT))[:, None, :], (L, 128, NT * 16)))
    m["dtb_rep"] = f(np.broadcast_to(np.tile(db, (1, NT))[:, None, :], (L, 128, NT * 16)))
    m["gnorm"] = f(np.asarray(inp["gdn_norm_w"][:L]).reshape(L, 128, 1))
    m["lamv"] = f(np.asarray(inp["diff_lambda"][:L]).reshape(L, 1, 256))
    m["dnorm"] = f(np.asarray(inp["diff_norm_w"][:L]).reshape(L, 128, 1))
    nb = []
    for l in range(L):
        bt, rm = _na_tables(np.asarray(inp["na_rpb"][l], dtype=np.float32), n_lat)
        nb.append(bt)
    m["na_bias"] = f(np.stack(nb, 0))
    m["rowmask"] = rm
    for nm in ("w_branch", "w_out", "ln_g", "ln_b", "w_router", "b_router", "w_exp1", "w_exp2", "b_exp2"):
        m[nm] = f(inp[nm][:L])
    m["b1T"] = f(np.asarray(inp["b_exp1"][:L]).reshape(L, 32, 16, 128).transpose(0, 1, 3, 2))
    m["ident"] = np.eye(128, dtype=np.float32)
    j = np.arange(128)[:, None]
    i = np.arange(128)[None, :]
    gm = np.empty((6, 128, 128), np.float32)
    for d, (inc, stc) in enumerate((((j <= i), (j < i)), ((j >= i), (j > i)))):
        gm[d * 3 + 0] = inc
        gm[d * 3 + 1] = np.where(inc, 0.0, NEG)
        gm[d * 3 + 2] = stc
    m["gmask"] = gm
    tm = np.zeros((2, 7, 128, 128), np.float32)
    for lv in range(7):
        bsz = 1 << lv
        up = ((j // (2 * bsz)) == (i // (2 * bsz))) & ((j // bsz) % 2 == 0) & ((i // bsz) % 2 == 1)
        tm[0, lv] = up
        tm[1, lv] = up.T
    m["tmask"] = tm
    m["cosT"], m["sinT"] = _rope_tables(n_lat)
    m["iota"] = f(np.broadcast_to(np.arange(32, dtype=np.float32)[None, :], (128, 32)))
    m["sut"] = f(j < i)
    return m


def moe_cap(T):
    return int(np.ceil(T * 4 / 32 * 1.2 / 128)) * 128


_NC_CACHE = {}


def run(inp, n_lat, n_ctx, L, nb):
    import math
    lam_inits = [0.8 - 0.6 * math.exp(-0.3 * l) for l in range(L)]
    T = n_lat + n_ctx
    cap = moe_cap(T)
    key = (n_lat, n_ctx, L)
    if key not in _NC_CACHE:
        _NC_CACHE[key] = build(n_lat, n_ctx, L, lam_inits, cap)
    nc = _NC_CACHE[key]
    in_maps = [prep(inp, b, n_lat, n_ctx, L) for b in range(nb)]
    res = run_bass_kernel_spmd(nc, in_maps, core_ids=list(range(nb)))
    if DEBUG_OUT:
        return res.results
    return np.stack([res.results[b]["out"][:n_lat] for b in range(nb)], 0)


def kernel(**inputs):
    inp = {k_: np.asarray(v) for k_, v in inputs.items()}
    return run(inp, 8192, 256, 4, 2).astype(np.float32)
```
